# Optimizing a Trainium2 kernel written in Bass

```python
import math
import jax, jax.numpy as jnp
from jax import lax
import numpy as np

D_MODEL = 1024
BATCH = 4
SEQ = 4096
DEPTH = 4

GRID_W = 64
CTX_LEN = 256
N_MIXERS = 2
N_SSM_LAYERS = (DEPTH + 1) // 2
N_ATTN_LAYERS = DEPTH // 2
SSM_GROUP = 16
SSM_GROUPS = D_MODEL // SSM_GROUP
SSM_STATE = 64
SSM_DT_MIN = 1e-3
SSM_DT_MAX = 1e-1
HEAD_DIM = 64
N_Q_HEADS = D_MODEL // HEAD_DIM
N_KV_HEADS = 4
Q_PER_KV = N_Q_HEADS // N_KV_HEADS
ROPE_THETA = 10000.0
Q_BLOCK = 128
D_FF = 2816
N_EXPERTS = 8
TOP_K = 2
N_MOD = 6
EPS = 1e-6

kernel_name = 'hybrid_s5_gqa_moe_prefix_dit'


def _rmsnorm(x, g):
    xf = x.astype(jnp.float32)
    y = xf * lax.rsqrt(jnp.mean(xf * xf, axis=-1, keepdims=True) + EPS) * g.astype(jnp.float32)
    return y.astype(x.dtype)


def _modulation(cvec, w, b):
    return jnp.split(jax.nn.silu(cvec) @ w + b, N_MOD, axis=-1)


def _modulate(h, shift, scale):
    return h * (1.0 + scale) + shift


def _zoh(a_re, a_im, log_dt, b_re, b_im):
    a_re = a_re.astype(jnp.float32)
    a_im = a_im.astype(jnp.float32)
    dt = jnp.exp(log_dt.astype(jnp.float32))[:, None]
    mag = jnp.exp(a_re * dt)
    lam_re = mag * jnp.cos(a_im * dt)
    lam_im = mag * jnp.sin(a_im * dt)
    den = a_re * a_re + a_im * a_im
    num_re = lam_re - 1.0
    f_re = (num_re * a_re + lam_im * a_im) / den
    f_im = (lam_im * a_re - num_re * a_im) / den
    b_re = b_re.astype(jnp.float32)
    b_im = b_im.astype(jnp.float32)
    bbar_re = f_re[..., None] * b_re - f_im[..., None] * b_im
    bbar_im = f_re[..., None] * b_im + f_im[..., None] * b_re
    return lam_re, lam_im, bbar_re, bbar_im


def _ssm_combine(e1, e2):
    ar1, ai1, br1, bi1 = e1
    ar2, ai2, br2, bi2 = e2
    return (ar1 * ar2 - ai1 * ai2,
            ar1 * ai2 + ai1 * ar2,
            ar2 * br1 - ai2 * bi1 + br2,
            ar2 * bi1 + ai2 * br1 + bi2)


def _s5_scan(u, a_re, a_im, log_dt, b_re, b_im, s0, reverse):
    lam_re, lam_im, bbar_re, bbar_im = _zoh(a_re, a_im, log_dt, b_re, b_im)
    bu_re = jnp.einsum('bngi,gpi->bngp', u, bbar_re)
    bu_im = jnp.einsum('bngi,gpi->bngp', u, bbar_im)
    if s0 is not None:
        s0_re, s0_im = s0
        pos = -1 if reverse else 0
        bu_re = bu_re.at[:, pos].add(lam_re * s0_re - lam_im * s0_im)
        bu_im = bu_im.at[:, pos].add(lam_re * s0_im + lam_im * s0_re)
    n = u.shape[1]
    a_seq_re = jnp.broadcast_to(lam_re, (1, n) + lam_re.shape)
    a_seq_im = jnp.broadcast_to(lam_im, (1, n) + lam_im.shape)
    _, _, s_re, s_im = lax.associative_scan(
        _ssm_combine, (a_seq_re, a_seq_im, bu_re, bu_im), reverse=reverse, axis=1)
    return s_re, s_im


def _s5_readout(s_re, s_im, c_re, c_im):
    return (jnp.einsum('bngp,gip->bngi', s_re, c_re.astype(jnp.float32))
            - jnp.einsum('bngp,gip->bngi', s_im, c_im.astype(jnp.float32)))


def _s5_output(y, h, d, glu_w, glu_b):
    y = y + d.astype(jnp.float32) * h.astype(jnp.float32)
    z = jax.nn.gelu(y).astype(h.dtype)
    a, g = jnp.split(z @ glu_w + glu_b, 2, axis=-1)
    return a * jax.nn.sigmoid(g)


def _s5_mixer(hl, hc, a_re, a_im, log_dt, b_re, b_im, c_re, c_im, d, glu_w, glu_b, need_ctx):
    bsz, n_l, _ = hl.shape
    n_c = hc.shape[1]
    u_c = hc.astype(jnp.float32).reshape(bsz, n_c, SSM_GROUPS, SSM_GROUP)
    u_l = hl.astype(jnp.float32).reshape(bsz, n_l, SSM_GROUPS, SSM_GROUP)
    y_l = jnp.zeros((bsz, n_l, D_MODEL), jnp.float32)
    y_c = jnp.zeros((bsz, n_c, D_MODEL), jnp.float32)
    for r in range(2):
        reverse = r == 1
        sc_re, sc_im = _s5_scan(u_c, a_re[r], a_im[r], log_dt[r], b_re[r], b_im[r], None, reverse)
        fin = (sc_re[:, 0], sc_im[:, 0]) if reverse else (sc_re[:, -1], sc_im[:, -1])
        sl_re, sl_im = _s5_scan(u_l, a_re[r], a_im[r], log_dt[r], b_re[r], b_im[r], fin, reverse)
        y_l = y_l + _s5_readout(sl_re, sl_im, c_re[r], c_im[r]).reshape(bsz, n_l, D_MODEL)
        if need_ctx:
            y_c = y_c + _s5_readout(sc_re, sc_im, c_re[r], c_im[r]).reshape(bsz, n_c, D_MODEL)
    out_l = _s5_output(y_l, hl, d, glu_w, glu_b)
    out_c = _s5_output(y_c, hc, d, glu_w, glu_b) if need_ctx else None
    return out_l, out_c


def _axial_rope_tables(rows):
    row = jnp.broadcast_to(jnp.arange(rows)[:, None], (rows, GRID_W)).reshape(-1).astype(jnp.float32)
    col = jnp.broadcast_to(jnp.arange(GRID_W)[None, :], (rows, GRID_W)).reshape(-1).astype(jnp.float32)
    axis_dim = HEAD_DIM // 2
    inv = ROPE_THETA ** (-jnp.arange(0, axis_dim, 2, dtype=jnp.float32) / axis_dim)
    ang = jnp.stack([row[:, None] * inv, col[:, None] * inv], axis=1)
    return jnp.cos(ang), jnp.sin(ang)


def _apply_rope(x, cos, sin):
    shp = x.shape
    xr = x.astype(jnp.float32).reshape(shp[:-1] + (2, 2, HEAD_DIM // 4))
    x1, x2 = xr[..., 0, :], xr[..., 1, :]
    cs, sn = cos[:, None], sin[:, None]
    out = jnp.stack([x1 * cs - x2 * sn, x1 * sn + x2 * cs], axis=-2)
    return out.reshape(shp).astype(x.dtype)


def _qkv(h, w_qkv, q_g, k_g):
    bsz, n, _ = h.shape
    qkv = h @ w_qkv
    q, k, v = jnp.split(qkv, [N_Q_HEADS * HEAD_DIM, (N_Q_HEADS + N_KV_HEADS) * HEAD_DIM], axis=-1)
    q = _rmsnorm(q.reshape(bsz, n, N_Q_HEADS, HEAD_DIM), q_g)
    k = _rmsnorm(k.reshape(bsz, n, N_KV_HEADS, HEAD_DIM), k_g)
    v = v.reshape(bsz, n, N_KV_HEADS, HEAD_DIM)
    return q, k, v


def _attend(q, k_parts, v_parts):
    scale = HEAD_DIM ** -0.5
    s = jnp.concatenate([jnp.einsum('bqkgd,bskd->bkgqs', q, kp) for kp in k_parts], axis=-1)
    p = jax.nn.softmax(s.astype(jnp.float32) * scale, axis=-1).astype(v_parts[0].dtype)
    o = None
    off = 0
    for vp in v_parts:
        sz = vp.shape[1]
        term = jnp.einsum('bkgqs,bskd->bqkgd', p[..., off:off + sz], vp)
        o = term if o is None else o + term
        off += sz
    return o


def _attn_mixer(hl, hc, w_qkv, q_g, k_g, w_o, cos, sin, need_ctx):
    bsz, n_l, _ = hl.shape
    n_c = hc.shape[1]
    ql, kl, vl = _qkv(hl, w_qkv, q_g, k_g)
    ql = _apply_rope(ql, cos, sin)
    kl = _apply_rope(kl, cos, sin)
    qc, kc, vc = _qkv(hc, w_qkv, q_g, k_g)
    n_blocks = n_l // Q_BLOCK
    qb = ql.reshape(bsz, n_blocks, Q_BLOCK, N_KV_HEADS, Q_PER_KV, HEAD_DIM).transpose(1, 0, 2, 3, 4, 5)
    ob = lax.map(lambda qblk: _attend(qblk, (kl, kc), (vl, vc)), qb)
    out_l = ob.transpose(1, 0, 2, 3, 4, 5).reshape(bsz, n_l, D_MODEL) @ w_o
    out_c = None
    if need_ctx:
        oc = _attend(qc.reshape(bsz, n_c, N_KV_HEADS, Q_PER_KV, HEAD_DIM), (kc,), (vc,))
        out_c = oc.reshape(bsz, n_c, D_MODEL) @ w_o
    return out_l, out_c


def _swiglu(h, w_gate_up, w_down):
    g, u = jnp.split(h @ w_gate_up, 2, axis=-1)
    return (jax.nn.silu(g) * u) @ w_down


def _moe(h, router_w, router_b, w_gate_up, w_down):
    logits = (h @ router_w).astype(jnp.float32) + router_b.astype(jnp.float32)
    top_v, top_i = lax.top_k(logits, TOP_K)
    top_w = jax.nn.softmax(top_v, axis=-1)
    gates = jnp.sum(jax.nn.one_hot(top_i, N_EXPERTS, dtype=jnp.float32) * top_w[..., None], axis=-2)
    gates = gates.astype(h.dtype)
    y = jnp.zeros_like(h)
    for e in range(N_EXPERTS):
        y = y + gates[..., e:e + 1] * _swiglu(h, w_gate_up[e], w_down[e])
    return y


def setup_inputs(seed: int = 0) -> dict:
    key = jax.random.key(seed)
    ks = jax.random.split(key, 28)
    f32 = jnp.float32
    D, G, P, I = D_MODEL, SSM_GROUPS, SSM_STATE, SSM_GROUP
    NS, NA = N_SSM_LAYERS, N_ATTN_LAYERS

    def nrm(k, shape, scale):
        return jax.random.normal(k, shape, f32) * scale

    n_idx = jnp.arange(P, dtype=f32)
    qkv_width = (N_Q_HEADS + 2 * N_KV_HEADS) * HEAD_DIM
    return {
        'x': nrm(ks[0], (BATCH, SEQ, D), 1.0),
        'c': nrm(ks[1], (BATCH, D), 1.0),
        'ctx': nrm(ks[2], (BATCH, CTX_LEN, D), 1.0),
        'c_ctx': nrm(ks[3], (D,), 1.0),
        'ada_w': nrm(ks[4], (DEPTH, D, N_MOD * D), 0.5 * D ** -0.5),
        'ada_b': nrm(ks[5], (DEPTH, N_MOD * D), 0.02),
        'norm_mix_g': 1.0 + nrm(ks[6], (DEPTH, D), 0.02),
        'norm_ffn_g': 1.0 + nrm(ks[7], (DEPTH, D), 0.02),
        'ssm_a_re': -0.5 + nrm(ks[8], (NS, 2, G, P), 0.01),
        'ssm_a_im': math.pi * n_idx + nrm(ks[9], (NS, 2, G, P), 0.01),
        'ssm_log_dt': jax.random.uniform(ks[10], (NS, 2, G), f32, math.log(SSM_DT_MIN), math.log(SSM_DT_MAX)),
        'ssm_b_re': nrm(ks[11], (NS, 2, G, P, I), (2 * I) ** -0.5),
        'ssm_b_im': nrm(ks[12], (NS, 2, G, P, I), (2 * I) ** -0.5),
        'ssm_c_re': nrm(ks[13], (NS, 2, G, I, P), (4 * P) ** -0.5),
        'ssm_c_im': nrm(ks[14], (NS, 2, G, I, P), (4 * P) ** -0.5),
        'ssm_d': nrm(ks[15], (NS, D), 0.5),
        'ssm_glu_w': nrm(ks[16], (NS, D, 2 * D), D ** -0.5),
        'ssm_glu_b': nrm(ks[17], (NS, 2 * D), 0.02),
        'attn_w_qkv': nrm(ks[18], (NA, D, qkv_width), D ** -0.5),
        'attn_q_g': 1.0 + nrm(ks[19], (NA, HEAD_DIM), 0.02),
        'attn_k_g': 1.0 + nrm(ks[20], (NA, HEAD_DIM), 0.02),
        'attn_w_o': nrm(ks[21], (NA, D, D), D ** -0.5),
        'ffn_w_gate_up': nrm(ks[22], (NS, D, 2 * D_FF), D ** -0.5),
        'ffn_w_down': nrm(ks[23], (NS, D_FF, D), D_FF ** -0.5),
        'moe_router_w': nrm(ks[24], (NA, D, N_EXPERTS), D ** -0.5),
        'moe_router_b': nrm(ks[25], (NA, N_EXPERTS), 0.01),
        'moe_w_gate_up': nrm(ks[26], (NA, N_EXPERTS, D, 2 * D_FF), D ** -0.5),
        'moe_w_down': nrm(ks[27], (NA, N_EXPERTS, D_FF, D), D_FF ** -0.5),
    }


def reference(x, c, ctx, c_ctx, ada_w, ada_b, norm_mix_g, norm_ffn_g,
              ssm_a_re, ssm_a_im, ssm_log_dt, ssm_b_re, ssm_b_im, ssm_c_re, ssm_c_im,
              ssm_d, ssm_glu_w, ssm_glu_b,
              attn_w_qkv, attn_q_g, attn_k_g, attn_w_o,
              ffn_w_gate_up, ffn_w_down,
              moe_router_w, moe_router_b, moe_w_gate_up, moe_w_down):
    n_lat = x.shape[1]
    rows = n_lat // GRID_W
    cos, sin = _axial_rope_tables(rows)
    xl, xc = x, ctx
    for i in range(DEPTH):
        j = i // 2
        last = i == DEPTH - 1
        ml = [m[:, None, :] for m in _modulation(c, ada_w[i], ada_b[i])]
        mc = _modulation(c_ctx, ada_w[i], ada_b[i])
        hl = _modulate(_rmsnorm(xl, norm_mix_g[i]), ml[0], ml[1])
        hc = _modulate(_rmsnorm(xc, norm_mix_g[i]), mc[0], mc[1])
        if i % N_MIXERS == 0:
            yl, yc = _s5_mixer(hl, hc, ssm_a_re[j], ssm_a_im[j], ssm_log_dt[j], ssm_b_re[j], ssm_b_im[j],
                               ssm_c_re[j], ssm_c_im[j], ssm_d[j], ssm_glu_w[j], ssm_glu_b[j],
                               need_ctx=not last)
        else:
            yl, yc = _attn_mixer(hl, hc, attn_w_qkv[j], attn_q_g[j], attn_k_g[j], attn_w_o[j],
                                 cos, sin, need_ctx=not last)
        xl = xl + ml[2] * yl
        if i % 2 == 0:
            ffn = _swiglu
            ffn_args = (ffn_w_gate_up[j], ffn_w_down[j])
        else:
            ffn = _moe
            ffn_args = (moe_router_w[j], moe_router_b[j], moe_w_gate_up[j], moe_w_down[j])
        hl = _modulate(_rmsnorm(xl, norm_ffn_g[i]), ml[3], ml[4])
        xl = xl + ml[5] * ffn(hl, *ffn_args)
        if not last:
            xc = xc + mc[2] * yc
            hc = _modulate(_rmsnorm(xc, norm_ffn_g[i]), mc[3], mc[4])
            xc = xc + mc[5] * ffn(hc, *ffn_args)
    return xl
```

```python
import contextlib
import numpy as np
import concourse.bass as bass
import concourse.mybir as mybir
from concourse.bass_utils import run_bass_kernel_spmd

F32 = mybir.dt.float32
BF16 = mybir.dt.bfloat16
I32 = mybir.dt.int32
AF = mybir.ActivationFunctionType
ALU = mybir.AluOpType

ENGS = ("pe", "act", "dve", "pool", "sp")
N_DMA_SEMS = 40

D = 1024
NCH = 8
T_CTX = 256
T_LAT = 4096
T = T_CTX + T_LAT
DFF = 2816
NFC = 22
NE = 8
EPS = 1e-6
TILES = [(0, 256, 1)] + [(256 + 512 * i, 512, 0) for i in range(8)]
TWO_PI = float(2 * np.pi)


class Emitter:
    def __init__(self, nc):
        self.nc = nc
        self.ins = {e: [] for e in ENGS}
        self.lastw = {}
        self.readers = {}
        self.clock = {e: {} for e in ENGS}
        self.dma_seen = {e: set() for e in ENGS}
        self.n_dma = 0
        self.dma_sem_last = [None] * N_DMA_SEMS
        self.dma_sem_cnt = [0] * N_DMA_SEMS
        self.final_dma = []

    def _need(self, eng, ev, waits):
        if ev is None:
            return
        if ev[0] == "c":
            _, f, idx = ev
            if f == eng and eng == "pe":
                return
            if self.clock[eng].get(f, -1) >= idx:
                return
            waits.append(ev)
        else:
            if ev[1] in self.dma_seen[eng]:
                return
            waits.append(ev)

    def _apply_waits(self, eng, waits):
        best = {}
        dm = {}
        for ev in waits:
            if ev[0] == "c":
                best[ev[1]] = max(best.get(ev[1], -1), ev[2])
            else:
                dm[ev[1]] = ev
        out = []
        for f, idx in best.items():
            out.append(("c", f, idx))
            self.ins[f][idx]["marked"] = True
            src = self.ins[f][idx]["clk"]
            for g, v in src.items():
                if self.clock[eng].get(g, -1) < v:
                    self.clock[eng][g] = v
            if self.clock[eng].get(f, -1) < idx:
                self.clock[eng][f] = idx
        for did, ev in dm.items():
            out.append(ev)
            self.dma_seen[eng].add(did)
        return out

    def _deps(self, eng, reads, writes):
        waits = []
        for k in reads:
            self._need(eng, self.lastw.get(k), waits)
        for k in writes:
            self._need(eng, self.lastw.get(k), waits)
            for ev in self.readers.get(k, ()):
                self._need(eng, ev, waits)
        return self._apply_waits(eng, waits)

    def _commit(self, ev, reads, writes):
        for k in reads:
            self.readers.setdefault(k, []).append(ev)
        for k in writes:
            self.lastw[k] = ev
            self.readers[k] = []

    def op(self, eng, fn, reads=(), writes=()):
        waits = self._deps(eng, reads, writes)
        idx = len(self.ins[eng])
        calls = []

        class _Rec:
            def __getattr__(self_, name):
                return lambda *a, **k: calls.append((name, a, k))
        fn(_Rec())
        assert len(calls) == 1, calls
        name, a, k = calls[0]
        fn = (lambda e, name=name, a=a, k=k: getattr(e, name)(*a, **k))
        self.ins[eng].append({"fn": fn, "waits": waits, "marked": False, "dma": None,
                              "clk": dict(self.clock[eng])})
        self._commit(("c", eng, idx), reads, writes)

    def dma(self, eng, out, in_, reads=(), writes=(), final=False, **kw):
        s = self.n_dma % N_DMA_SEMS
        waits = self._deps(eng, reads, writes)
        prev = self.dma_sem_last[s]
        if prev is not None and prev[1] not in self.dma_seen[eng]:
            waits.append(prev)
            self.dma_seen[eng].add(prev[1])
        self.dma_sem_cnt[s] += 1
        ev = ("d", self.n_dma, s, 16 * self.dma_sem_cnt[s])
        self.dma_sem_last[s] = ev
        self.n_dma += 1
        self.ins[eng].append({"fn": (lambda e, out=out, in_=in_, kw=kw: e.dma_start(out=out, in_=in_, **kw)),
                              "waits": waits, "marked": False, "dma": ev, "clk": dict(self.clock[eng])})
        self._commit(ev, reads, writes)
        if final:
            self.final_dma.append(ev)

    def fence(self):
        last = {}
        for e in ENGS:
            idx = len(self.ins[e]) - 1
            while idx >= 0 and (self.ins[e][idx]["dma"] is not None or self.ins[e][idx]["fn"] is None):
                idx -= 1
            if idx >= 0:
                last[e] = idx
        dmas = [ev for ev in self.dma_sem_last if ev is not None]
        for e in ENGS:
            waits = []
            for f, idx in last.items():
                if f == e:
                    continue
                if self.clock[e].get(f, -1) < idx:
                    waits.append(("c", f, idx))
                    self.ins[f][idx]["marked"] = True
                    self.clock[e][f] = idx
            for ev in dmas:
                if ev[1] not in self.dma_seen[e]:
                    waits.append(ev)
                    self.dma_seen[e].add(ev[1])
            self.ins[e].append({"fn": None, "waits": waits, "marked": False, "dma": None, "clk": dict(self.clock[e])})
        for e in ENGS:
            for f, idx in last.items():
                self.clock[e][f] = max(self.clock[e].get(f, -1), idx)
        self.lastw = {}
        self.readers = {}

    def emit(self):
        nc = self.nc
        waits = []
        for ev in self.final_dma:
            if ev[1] not in self.dma_seen["sp"]:
                waits.append(ev)
        for e in ENGS:
            if e != "sp":
                idx = len(self.ins[e]) - 1
                while idx >= 0 and (self.ins[e][idx]["dma"] is not None or self.ins[e][idx]["fn"] is None):
                    idx -= 1
                if idx >= 0:
                    self.ins[e][idx]["marked"] = True
                    waits.append(("c", e, idx))
        self.ins["sp"].append({"fn": None, "waits": waits, "marked": False, "dma": None, "clk": {}})
        semval = {}
        for e in ENGS:
            c = 0
            for i, rec in enumerate(self.ins[e]):
                if rec["marked"]:
                    c += 1
                    semval[(e, i)] = c
        with contextlib.ExitStack() as st:
            esem = {e: st.enter_context(nc.semaphore("s_" + e)) for e in ENGS}
            dsem = [st.enter_context(nc.semaphore("d_%d" % i)) for i in range(N_DMA_SEMS)]
            block = st.enter_context(nc.Block())
            engobj = {"pe": block.tensor, "act": block.scalar, "dve": block.vector,
                      "pool": block.gpsimd, "sp": block.sync}

            def make(e):
                def body(eng):
                    for i, rec in enumerate(self.ins[e]):
                        for ev in rec["waits"]:
                            if ev[0] == "c":
                                eng.wait_ge(esem[ev[1]], semval[(ev[1], ev[2])])
                            else:
                                eng.wait_ge(dsem[ev[2]], ev[3])
                        if rec["fn"] is None:
                            continue
                        r = rec["fn"](eng)
                        if rec["dma"] is not None:
                            r.then_inc(dsem[rec["dma"][2]], 16)
                        elif rec["marked"]:
                            r.then_inc(esem[e], 1)
                return body
            for e in ENGS:
                engobj[e](make(e))
        return {e: len(self.ins[e]) for e in ENGS}


def _consts():
    ident = np.eye(128, dtype=np.float32)
    maskB = np.zeros((4, 128, 128), np.float32)
    for q in range(4):
        for gl in range(2):
            gq = 2 * q + gl
            maskB[q, gl * 64:(gl + 1) * 64, gq * 16:(gq + 1) * 16] = 1.0
    maskC = np.ascontiguousarray(maskB.transpose(0, 2, 1))
    R = np.zeros((64, 64), np.float32)
    for blk in range(2):
        for d in range(16):
            R[blk * 32 + d, blk * 32 + d + 16] = -1.0
            R[blk * 32 + d + 16, blk * 32 + d] = 1.0
    rotT = np.ascontiguousarray(R.T)
    rows = T_LAT // 64
    row = np.repeat(np.arange(rows), 64).astype(np.float32)
    col = np.tile(np.arange(64), rows).astype(np.float32)
    inv = (10000.0 ** (-np.arange(0, 32, 2, dtype=np.float32) / 32)).astype(np.float32)
    ang_r = row[None, :] * inv[:, None]
    ang_c = col[None, :] * inv[:, None]
    cosF = np.concatenate([np.cos(ang_r), np.cos(ang_r), np.cos(ang_c), np.cos(ang_c)], 0).astype(np.float32)
    sinF = np.concatenate([np.sin(ang_r), np.sin(ang_r), np.sin(ang_c), np.sin(ang_c)], 0).astype(np.float32)
    return {"k_ident": ident, "k_maskB": maskB, "k_maskC": maskC, "k_rotT": rotT,
            "k_cosF": np.ascontiguousarray(cosF), "k_sinF": np.ascontiguousarray(sinF)}


IN_SHAPES = {
    "x": [T_LAT, D], "c": [1, D], "ctx": [T_CTX, D], "c_ctx": [1, D],
    "ada_w": [4, D, 6 * D], "ada_b": [4, 6 * D], "norm_mix_g": [4, D], "norm_ffn_g": [4, D],
    "ssm_a_re": [2, 2, 64, 64], "ssm_a_im": [2, 2, 64, 64], "ssm_log_dt": [2, 2, 64],
    "ssm_b_re": [2, 2, 64, 64, 16], "ssm_b_im": [2, 2, 64, 64, 16],
    "ssm_c_re": [2, 2, 64, 16, 64], "ssm_c_im": [2, 2, 64, 16, 64],
    "ssm_d": [2, D], "ssm_glu_w": [2, D, 2 * D], "ssm_glu_b": [2, 2 * D],
    "attn_w_qkv": [2, D, 1536], "attn_q_g": [2, 64], "attn_k_g": [2, 64], "attn_w_o": [2, D, D],
    "ffn_w_gate_up": [2, D, 2 * DFF], "ffn_w_down": [2, DFF, D],
    "moe_router_w": [2, D, NE], "moe_router_b": [2, NE],
    "moe_w_gate_up": [2, NE, D, 2 * DFF], "moe_w_down": [2, NE, DFF, D],
    "k_ident": [128, 128], "k_maskB": [4, 128, 128], "k_maskC": [4, 128, 128], "k_rotT": [64, 64],
    "k_cosF": [64, T_LAT], "k_sinF": [64, T_LAT],
}


def build(stop_after=None):
    nc = bass.Bass("TRN2", target_bir_lowering=False)
    dr = {k: nc.dram_tensor(k, s, F32, kind="ExternalInput").ap() for k, s in IN_SHAPES.items()}
    out_d = nc.dram_tensor("out", [T_LAT, D], F32, kind="ExternalOutput").ap()
    xT = nc.dram_tensor("xT_scr", [NCH, 128, T], F32, kind="Internal").ap()
    DBG = stop_after is not None
    K = Emitter(nc)
    st = contextlib.ExitStack()

    def sb(name, shape, dt=F32):
        return st.enter_context(nc.sbuf_tensor(name, shape, dt))

    ps = [st.enter_context(nc.psum_tensor("ps%d" % i, [128, 512], F32)) for i in range(8)]
    PS = ["ps%d" % i for i in range(8)]

    hT = sb("hT", [128, NCH, T], BF16)
    ident = sb("ident", [128, 128])
    ones_b = sb("ones_b", [128, 128], BF16)
    ones_f = sb("ones_f", [128, 512])
    iota_f = sb("iota_f", [128, 512])
    iota_i = sb("iota_i", [128, 512], I32)
    modv = sb("modv", [128, 4, 6, NCH, 2])
    Amix = sb("Amix", [128, 4, NCH, 2]); Affn = sb("Affn", [128, 4, NCH, 2])
    gmix = sb("gmix", [128, 4, NCH]); gffn = sb("gffn", [128, 4, NCH])
    adab = sb("adab", [128, 4, 48])
    csil = sb("csil", [128, NCH, 2]); csil_b = sb("csil_b", [128, NCH, 2], BF16)
    epsb = sb("epsb", [128, 1])
    NA = 32256
    ARENA = sb("ARENA", [128, NA])

    class Al:
        def __init__(self):
            self.o = 0

        def f(self, n):
            a = ARENA[:, self.o:self.o + n]
            self.o += n
            assert self.o <= NA, self.o
            return a

        def b(self, n):
            w = (n + 1) // 2
            return self.f(w).bitcast(BF16)[:, 0:n]

        def i(self, n):
            return self.f(n).bitcast(I32)

    def dmaq(i):
        return "sp" if i % 2 == 0 else "act"

    K.dma("sp", ident[:], dr["k_ident"][:, :], writes=["ident"])
    K.op("dve", lambda e: e.memset(ones_b[:], 1.0), writes=["ones_b"])
    K.op("dve", lambda e: e.memset(ones_f[:], 1.0), writes=["ones_f"])
    K.op("dve", lambda e: e.memset(epsb[:], EPS), writes=["epsb"])
    K.op("pool", lambda e: e.iota(iota_i[:], pattern=[[1, 512]], base=1, channel_multiplier=0), writes=["iota_i"])
    K.op("dve", lambda e: e.tensor_copy(out=iota_f[:], in_=iota_i[:]), reads=["iota_i"], writes=["iota_f"])

    al = Al()
    xin = [al.f(1024) for i in range(2)]
    xo = [al.f(1024).rearrange("p (c t) -> p c t", c=NCH) for i in range(2)]
    for blk in range(T // 128):
        b2 = blk % 2
        src = dr["ctx"][blk * 128:(blk + 1) * 128, :] if blk < 2 else dr["x"][(blk - 2) * 128:(blk - 1) * 128, :]
        K.dma(dmaq(blk), xin[b2], src, writes=[("xin", b2)])
        for half in range(2):
            bank = 2 * b2 + half
            for j in range(4):
                c = half * 4 + j
                K.op("pe", lambda e, bank=bank, j=j, c=c, b2=b2: e.transpose(
                    ps[bank][:, j * 128:(j + 1) * 128], xin[b2][:, c * 128:(c + 1) * 128], ident[:]),
                    reads=[("xin", b2), "ident"], writes=[PS[bank]])
            eng = "act" if half == 0 else "dve"
            if eng == "act":
                K.op("act", lambda e, bank=bank, half=half, b2=b2: e.activation(
                    out=xo[b2][:, half * 4:(half + 1) * 4, :], in_=ps[bank][:].rearrange("p (c t) -> p c t", c=4), func=AF.Copy),
                    reads=[PS[bank]], writes=[("xo", b2, half)])
            else:
                K.op("dve", lambda e, bank=bank, half=half, b2=b2: e.tensor_copy(
                    out=xo[b2][:, half * 4:(half + 1) * 4, :], in_=ps[bank][:].rearrange("p (c t) -> p c t", c=4)),
                    reads=[PS[bank]], writes=[("xo", b2, half)])
        K.dma(dmaq(blk + 1), xT[:, :, blk * 128:(blk + 1) * 128].rearrange("c p t -> p c t"), xo[b2],
              reads=[("xo", b2, 0), ("xo", b2, 1)], writes=[("xT", blk // 4 if blk >= 2 else -1)])
    K.fence()

    cs_f = csil
    K.dma("sp", cs_f[:, :, 0], dr["c"][0, :].rearrange("(k p) -> p k", p=128), writes=["csil"], allow_slow_non_contiguous=True)
    K.dma("sp", cs_f[:, :, 1], dr["c_ctx"][0, :].rearrange("(k p) -> p k", p=128), writes=["csil"], allow_slow_non_contiguous=True)
    K.op("act", lambda e: e.activation(out=csil_b[:], in_=csil[:], func=AF.Silu), reads=["csil"], writes=["csil_b"])
    K.dma("sp", adab[:], dr["ada_b"].rearrange("l (j p) -> p l j", p=128), writes=["adab"], allow_slow_non_contiguous=True)
    K.dma("sp", gmix[:], dr["norm_mix_g"].rearrange("l (j p) -> p l j", p=128), writes=["gmix"], allow_slow_non_contiguous=True)
    K.dma("sp", gffn[:], dr["norm_ffn_g"].rearrange("l (j p) -> p l j", p=128), writes=["gffn"], allow_slow_non_contiguous=True)
    al = Al()
    wada = [al.b(8192).rearrange("p (k o) -> p k o", k=NCH) for i in range(2)]
    it = 0
    for l in range(4):
        for m in range(6):
            w2 = it % 2
            K.dma("pool", wada[w2], dr["ada_w"][l, :, m * 1024:(m + 1) * 1024].rearrange("(k p) o -> p k o", p=128),
                  writes=[("wada", w2)])
            bank = it % 2
            for cc in range(NCH):
                for k in range(NCH):
                    K.op("pe", lambda e, bank=bank, cc=cc, k=k, w2=w2: e.matmul(
                        ps[bank][:, cc * 2:cc * 2 + 2], lhsT=wada[w2][:, k, cc * 128:(cc + 1) * 128], rhs=csil_b[:, k, :],
                        start=(k == 0), stop=(k == NCH - 1)), reads=[("wada", w2), "csil_b"], writes=[PS[bank]])
            for col in range(2):
                K.op("dve", lambda e, bank=bank, l=l, m=m, col=col: e.tensor_tensor(
                    out=modv[:, l, m, :, col], in0=ps[bank][:, 0:16].rearrange("p (c t) -> p c t", t=2)[:, :, col],
                    in1=adab[:, l, m * 8:(m + 1) * 8], op=ALU.add), reads=[PS[bank], "adab"], writes=["modv"])
            it += 1
    for l in range(4):
        for col in range(2):
            K.op("dve", lambda e, l=l, col=col: e.scalar_tensor_tensor(
                out=Amix[:, l, :, col], in0=modv[:, l, 1, :, col], scalar=1.0, in1=gmix[:, l, :], op0=ALU.add, op1=ALU.mult),
                reads=["modv", "gmix"], writes=["Amix"])
            K.op("dve", lambda e, l=l, col=col: e.scalar_tensor_tensor(
                out=Affn[:, l, :, col], in0=modv[:, l, 4, :, col], scalar=1.0, in1=gffn[:, l, :], op0=ALU.add, op1=ALU.mult),
                reads=["modv", "gffn"], writes=["Affn"])
    K.fence()

    def norm_phase(l, A, shift_m, router=None):
        al = Al()
        xt = [al.f(4096).rearrange("p (c t) -> p c t", c=NCH) for i in range(2)]
        sq = [al.b(4096).rearrange("p (c t) -> p c t", c=NCH) for i in range(2)]
        ms = al.f(512); rinv = al.f(512); rstd = al.f(512)
        tmp = [al.f(512) for i in range(2)]
        for ti, (t0, n, col) in enumerate(TILES):
            b2 = ti % 2
            K.dma(dmaq(ti), xt[b2][:, :, :n], xT[:, :, t0:t0 + n].rearrange("c p t -> p c t"),
                  reads=[("xT", ti - 1)], writes=[("xt", b2)])
            K.op("act", lambda e, b2=b2, n=n: e.activation(out=sq[b2][:, :, :n], in_=xt[b2][:, :, :n], func=AF.Square),
                 reads=[("xt", b2)], writes=[("sq", b2)])
            for c in range(NCH):
                K.op("pe", lambda e, b2=b2, n=n, c=c: e.matmul(ps[b2][:, :n], lhsT=ones_b[:], rhs=sq[b2][:, c, :n],
                                                             start=(c == 0), stop=(c == NCH - 1)),
                     reads=[("sq", b2), "ones_b"], writes=[PS[b2]])
            K.op("dve", lambda e, b2=b2, n=n: e.tensor_scalar(out=ms[:, :n], in0=ps[b2][:, :n], scalar1=1.0 / D, scalar2=EPS,
                                                              op0=ALU.mult, op1=ALU.add), reads=[PS[b2]], writes=["ms"])
            K.op("dve", lambda e, n=n: e.reciprocal(out=rinv[:, :n], in_=ms[:, :n]), reads=["ms"], writes=["rinv"])
            K.op("act", lambda e, n=n: e.activation(out=rstd[:, :n], in_=rinv[:, :n], func=AF.Sqrt), reads=["rinv"], writes=["rstd"])
            for c in range(NCH):
                c2 = c % 2
                K.op("dve", lambda e, b2=b2, n=n, c=c, c2=c2, col=col: e.scalar_tensor_tensor(
                    out=tmp[c2][:, :n], in0=xt[b2][:, c, :n], scalar=A[:, l, c, col:col + 1], in1=rstd[:, :n],
                    op0=ALU.mult, op1=ALU.mult), reads=[("xt", b2), "rstd"], writes=[("tmp", c2)])
                K.op("act", lambda e, n=n, c=c, c2=c2, col=col, t0=t0: e.activation(
                    out=hT[:, c, t0:t0 + n], in_=tmp[c2][:, :n], func=AF.Identity,
                    bias=modv[:, l, shift_m, c, col:col + 1], scale=1.0), reads=[("tmp", c2)], writes=[("hT", ti)])
        K.fence()

    def resid_update(ti, t0, n, col, c, psb, gate_m, l, xold, xnew, kx):
        K.op("dve", lambda e: e.scalar_tensor_tensor(
            out=xnew[:, c, :n], in0=ps[psb][:, :n], scalar=modv[:, l, gate_m, c, col:col + 1], in1=xold[:, c, :n],
            op0=ALU.mult, op1=ALU.add), reads=[PS[psb], kx], writes=[kx + ("n",)])

    def s5_phase(l):
        j = l // 2
        al = Al()
        fa = al.f
        fb = al.b
        BC = [[fa(128) for _ in range(4)] for _ in range(2)]
        mkB = fa(512); mkC = fa(512)
        PRM0 = al.o
        prm = {nm: fa(64) for nm in ["a_re", "a_im", "ldt", "dt", "mag", "th", "thn", "t0", "t1", "t2", "sn", "cs",
                                      "lre", "lim", "den", "rden", "nr", "f_re", "f_im", "thn2"]}
        prm_i = al.i(64)
        dvec = fa(8)
        yacc = fa(T)
        tabC = [fa(512) for _ in range(4)]; tabS = [fa(512) for _ in range(4)]; tabM = [fa(512) for _ in range(4)]
        tw = [fa(512) for _ in range(10)]
        tint = al.i(512)
        init = fa(8 * 4).rearrange("p (g k) -> p g k", g=4)
        padt = [fa(128) for _ in range(4)]
        lhs = [[fb(128) for _ in range(4)] for _ in range(4)]
        s_re = [fb(512) for _ in range(2)]; s_im = [fb(512) for _ in range(2)]

        for gl in range(2):
            sl = slice(gl * 64, (gl + 1) * 64)
            for nm in ("ssm_a_re", "ssm_a_im"):
                K.dma("sp", prm[nm[4:]][sl, :].rearrange("p (r gp) -> p r gp", r=2),
                      dr[nm][j].rearrange("r (gp gl) p -> gl p r gp", gl=2)[gl], writes=[nm[4:]], allow_slow_non_contiguous=True)
            ldt_t = dr["ssm_log_dt"].tensor
            for r in range(2):
                src = bass.AP(ldt_t, j * 128 + r * 64 + gl, [[0, 64], [2, 32]])
                K.dma("sp", prm["ldt"][sl, r * 32:(r + 1) * 32], src, writes=["ldt"], allow_slow_non_contiguous=True)
        K.dma("sp", mkB.rearrange("p (q c) -> p q c", q=4), dr["k_maskB"].rearrange("q p c -> p q c"), writes=["mkB"])
        K.dma("sp", mkC.rearrange("p (q c) -> p q c", q=4), dr["k_maskC"].rearrange("q p c -> p q c"), writes=["mkC"])
        K.dma("sp", dvec, dr["ssm_d"][j].rearrange("(c p) -> p c", p=128), writes=["dvec"], allow_slow_non_contiguous=True)

        P = prm

        def dv(fn, r, w):
            K.op("dve", fn, reads=r, writes=w)

        def frac_sin(src, dst, tag):
            dv(lambda e: e.tensor_copy(out=prm_i, in_=src), [tag], ["prm_i"])
            dv(lambda e: e.tensor_copy(out=P["t0"], in_=prm_i), ["prm_i"], ["t0"])
            dv(lambda e: e.tensor_tensor(out=P["t1"], in0=src, in1=P["t0"], op=ALU.subtract), [tag, "t0"], ["t1"])
            K.op("act", lambda e: e.activation(out=dst, in_=P["t1"], func=AF.Sin, scale=TWO_PI), reads=["t1"], writes=[tag + "_o"])
        K.op("act", lambda e: e.activation(out=P["dt"], in_=P["ldt"], func=AF.Exp), reads=["ldt"], writes=["dt"])
        dv(lambda e: e.tensor_tensor(out=P["t2"], in0=P["a_re"], in1=P["dt"], op=ALU.mult), ["a_re", "dt"], ["t2"])
        K.op("act", lambda e: e.activation(out=P["mag"], in_=P["t2"], func=AF.Exp), reads=["t2"], writes=["mag"])
        dv(lambda e: e.tensor_tensor(out=P["th"], in0=P["a_im"], in1=P["dt"], op=ALU.mult), ["a_im", "dt"], ["th"])
        dv(lambda e: e.tensor_scalar(out=P["thn"], in0=P["th"], scalar1=1.0 / TWO_PI, scalar2=None, op0=ALU.mult), ["th"], ["thn"])
        dv(lambda e: e.tensor_scalar(out=P["thn2"], in0=P["thn"], scalar1=0.25, scalar2=None, op0=ALU.add), ["thn"], ["thn2"])
        frac_sin(P["thn"], P["sn"], "thn")
        frac_sin(P["thn2"], P["cs"], "thn2")
        dv(lambda e: e.tensor_tensor(out=P["lre"], in0=P["mag"], in1=P["cs"], op=ALU.mult), ["mag", "thn2_o"], ["lre"])
        dv(lambda e: e.tensor_tensor(out=P["lim"], in0=P["mag"], in1=P["sn"], op=ALU.mult), ["mag", "thn_o"], ["lim"])
        dv(lambda e: e.tensor_tensor(out=P["t0"], in0=P["a_re"], in1=P["a_re"], op=ALU.mult), ["a_re", "t1"], ["t0"])
        dv(lambda e: e.tensor_tensor(out=P["t1"], in0=P["a_im"], in1=P["a_im"], op=ALU.mult), ["a_im", "t0"], ["t1"])
        dv(lambda e: e.tensor_tensor(out=P["den"], in0=P["t0"], in1=P["t1"], op=ALU.add), ["t0", "t1"], ["den"])
        dv(lambda e: e.reciprocal(out=P["rden"], in_=P["den"]), ["den"], ["rden"])
        dv(lambda e: e.tensor_scalar(out=P["nr"], in0=P["lre"], scalar1=-1.0, scalar2=None, op0=ALU.add), ["lre"], ["nr"])
        dv(lambda e: e.tensor_tensor(out=P["t0"], in0=P["nr"], in1=P["a_re"], op=ALU.mult), ["nr", "a_re", "den"], ["t0"])
        dv(lambda e: e.tensor_tensor(out=P["t1"], in0=P["lim"], in1=P["a_im"], op=ALU.mult), ["lim", "a_im", "den"], ["t1"])
        dv(lambda e: e.tensor_tensor(out=P["t2"], in0=P["t0"], in1=P["t1"], op=ALU.add), ["t0", "t1", "mag"], ["t2"])
        dv(lambda e: e.tensor_tensor(out=P["f_re"], in0=P["t2"], in1=P["rden"], op=ALU.mult), ["t2", "rden"], ["f_re"])
        dv(lambda e: e.tensor_tensor(out=P["t0"], in0=P["lim"], in1=P["a_re"], op=ALU.mult), ["lim", "a_re", "t2"], ["t0"])
        dv(lambda e: e.tensor_tensor(out=P["t1"], in0=P["nr"], in1=P["a_im"], op=ALU.mult), ["nr", "a_im", "t2"], ["t1"])
        dv(lambda e: e.tensor_tensor(out=P["t2"], in0=P["t0"], in1=P["t1"], op=ALU.subtract), ["t0", "t1", "f_re"], ["t2"])
        dv(lambda e: e.tensor_tensor(out=P["f_im"], in0=P["t2"], in1=P["rden"], op=ALU.mult), ["t2", "rden"], ["f_im"])

        (br, bi, q1, q2, q3, q4, rin_re, rin_im, r_re, r_im) = tw
        u1, u2, u3, u4 = q1, q2, q3, q4

        for cc in range(NCH):
            for r in range(2):
                order = list(range(len(TILES))) if r == 0 else [0] + list(range(len(TILES) - 1, 0, -1))
                bcp = (cc * 2 + r) % 2
                Bre, Bim, Cre, Cim = BC[bcp]
                kbc = ("BC", bcp)
                for gl in range(2):
                    sl = slice(gl * 64, (gl + 1) * 64)
                    for nm, dst in (("ssm_b_re", Bre), ("ssm_b_im", Bim)):
                        K.dma("sp", dst[sl, :].rearrange("p (g i) -> p g i", i=16),
                              dr[nm][j, r, cc * 8:(cc + 1) * 8].rearrange("g p i -> p g i"), writes=[kbc], allow_slow_non_contiguous=True)
                    for nm, dst in (("ssm_c_re", Cre), ("ssm_c_im", Cim)):
                        K.dma("act", dst[:, gl * 64:(gl + 1) * 64],
                              dr[nm][j, r, cc * 8:(cc + 1) * 8].rearrange("g i p -> (g i) p"), writes=[kbc])
                for gpl in range(4):
                    gp = cc * 4 + gpl
                    cb = r * 32 + gp
                    fre = P["f_re"][:, cb:cb + 1]; fim = P["f_im"][:, cb:cb + 1]
                    mB = mkB[:, gpl * 128:(gpl + 1) * 128]; mC = mkC[:, gpl * 128:(gpl + 1) * 128]
                    for which in range(4):
                        pt = padt[which]
                        kp = ("padt", which)
                        if which == 0:
                            dv(lambda e, Bim=Bim, fim=fim, pt=pt: e.tensor_scalar(out=pt, in0=Bim, scalar1=fim, scalar2=None, op0=ALU.mult),
                               [kbc, "f_im"], [kp])
                            dv(lambda e, Bre=Bre, fre=fre, pt=pt: e.scalar_tensor_tensor(out=pt, in0=Bre, scalar=fre, in1=pt, op0=ALU.mult, op1=ALU.subtract),
                               [kbc, "f_re", kp], [kp])
                            dv(lambda e, pt=pt, mB=mB: e.tensor_tensor(out=pt, in0=pt, in1=mB, op=ALU.mult), [kp, "mkB"], [kp])
                        elif which == 1:
                            dv(lambda e, Bre=Bre, fim=fim, pt=pt: e.tensor_scalar(out=pt, in0=Bre, scalar1=fim, scalar2=None, op0=ALU.mult),
                               [kbc, "f_im"], [kp])
                            dv(lambda e, Bim=Bim, fre=fre, pt=pt: e.scalar_tensor_tensor(out=pt, in0=Bim, scalar=fre, in1=pt, op0=ALU.mult, op1=ALU.add),
                               [kbc, "f_re", kp], [kp])
                            dv(lambda e, pt=pt, mB=mB: e.tensor_tensor(out=pt, in0=pt, in1=mB, op=ALU.mult), [kp, "mkB"], [kp])
                        elif which == 2:
                            dv(lambda e, pt=pt, Cre=Cre, mC=mC: e.tensor_tensor(out=pt, in0=Cre, in1=mC, op=ALU.mult), [kbc, "mkC"], [kp])
                        else:
                            dv(lambda e, pt=pt, Cim=Cim, mC=mC: e.tensor_tensor(out=pt, in0=Cim, in1=mC, op=ALU.mult), [kbc, "mkC"], [kp])
                        K.op("pe", lambda e, pt=pt: e.transpose(ps[6][:, 0:128], pt, ident[:]), reads=[kp, "ident"], writes=[PS[6]])
                        K.op("act", lambda e, gpl=gpl, which=which: e.activation(
                            out=lhs[gpl][which], in_=ps[6][:, 0:128], func=AF.Copy, scale=(-1.0 if which == 3 else 1.0)),
                            reads=[PS[6]], writes=[("lhs", gpl, which)])
                    thn = P["thn"][:, cb:cb + 1]
                    for tab, off, nm in ((tabS[gpl], 0.0, "S"), (tabC[gpl], 0.25, "C")):
                        kt = ("tab", nm, gpl)
                        dv(lambda e, thn=thn, off=off: e.tensor_scalar(out=q1, in0=iota_f[:], scalar1=thn, scalar2=off, op0=ALU.mult, op1=ALU.add),
                           ["iota_f", "thn"], ["q1"])
                        dv(lambda e: e.tensor_copy(out=tint, in_=q1), ["q1"], ["tint"])
                        dv(lambda e: e.tensor_copy(out=q2, in_=tint), ["tint"], ["q2"])
                        dv(lambda e: e.tensor_tensor(out=q3, in0=q1, in1=q2, op=ALU.subtract), ["q1", "q2"], ["q3"])
                        K.op("act", lambda e, tab=tab: e.activation(out=tab, in_=q3, func=AF.Sin, scale=TWO_PI), reads=["q3"], writes=[kt])
                    K.op("pool", lambda e, gpl=gpl, cb=cb: e.tensor_scalar(out=tabM[gpl], in0=ones_f[:], scalar1=P["mag"][:, cb:cb + 1],
                                                                         scalar2=0.0, op0=ALU.mult, op1=ALU.add),
                         reads=["ones_f", "mag"], writes=[("tab", "M", gpl)])
                for wi, ti in enumerate(order):
                    t0, n, col = TILES[ti]
                    rev = (r == 1)
                    yb = 4 + (wi % 2)
                    for gpl in range(4):
                        b2 = gpl % 2
                        Cc = tabC[gpl][:, :n]; Ss = tabS[gpl][:, :n]; Mm = tabM[gpl][:, :n]
                        kC = ("tab", "C", gpl); kS = ("tab", "S", gpl); kM = ("tab", "M", gpl)
                        for half, bank in ((0, 2 * b2), (1, 2 * b2 + 1)):
                            K.op("pe", lambda e, bank=bank, gpl=gpl, half=half, t0=t0, n=n: e.matmul(
                                ps[bank][:, :n], lhsT=lhs[gpl][half], rhs=hT[:, cc, t0:t0 + n], start=True, stop=True),
                                reads=[("lhs", gpl, half), ("hT", ti)], writes=[PS[bank]])
                        src_re = ps[2 * b2][:, :n]; src_im = ps[2 * b2 + 1][:, :n]
                        if rev:
                            src_re = src_re[:, ::-1]; src_im = src_im[:, ::-1]
                        K.op("act", lambda e, s=src_re, n=n: e.activation(out=br[:, :n], in_=s, func=AF.Copy), reads=[PS[2 * b2]], writes=["br"])
                        K.op("act", lambda e, s=src_im, n=n: e.activation(out=bi[:, :n], in_=s, func=AF.Copy), reads=[PS[2 * b2 + 1]], writes=["bi"])
                        K.op("pool", lambda e, n=n, Cc=Cc: e.tensor_tensor(out=q1[:, :n], in0=br[:, :n], in1=Cc, op=ALU.mult), reads=["br", kC], writes=["q1"])
                        K.op("pool", lambda e, n=n, Ss=Ss: e.tensor_tensor(out=q2[:, :n], in0=bi[:, :n], in1=Ss, op=ALU.mult), reads=["bi", kS], writes=["q2"])
                        dv(lambda e, n=n, Cc=Cc: e.tensor_tensor(out=q3[:, :n], in0=bi[:, :n], in1=Cc, op=ALU.mult), ["bi", kC], ["q3"])
                        dv(lambda e, n=n, Ss=Ss: e.tensor_tensor(out=q4[:, :n], in0=br[:, :n], in1=Ss, op=ALU.mult), ["br", kS], ["q4"])
                        K.op("pool", lambda e, n=n: e.tensor_tensor(out=rin_re[:, :n], in0=q1[:, :n], in1=q2[:, :n], op=ALU.add), reads=["q1", "q2"], writes=["rin_re"])
                        dv(lambda e, n=n: e.tensor_tensor(out=rin_im[:, :n], in0=q3[:, :n], in1=q4[:, :n], op=ALU.subtract), ["q3", "q4"], ["rin_im"])
                        ki = ("init", gpl)
                        if wi == 0:
                            dv(lambda e, n=n, Mm=Mm: e.tensor_tensor_scan(out=r_re[:, :n], data0=Mm, data1=rin_re[:, :n], initial=0.0, op0=ALU.mult, op1=ALU.add),
                               [kM, "rin_re"], ["r_re"])
                            dv(lambda e, n=n, Mm=Mm: e.tensor_tensor_scan(out=r_im[:, :n], data0=Mm, data1=rin_im[:, :n], initial=0.0, op0=ALU.mult, op1=ALU.add),
                               [kM, "rin_im"], ["r_im"])
                        else:
                            dv(lambda e, n=n, Mm=Mm, gpl=gpl: e.tensor_tensor_scan(out=r_re[:, :n], data0=Mm, data1=rin_re[:, :n], initial=init[:, gpl, 0:1], op0=ALU.mult, op1=ALU.add),
                               [kM, "rin_re", ki], ["r_re"])
                            dv(lambda e, n=n, Mm=Mm, gpl=gpl: e.tensor_tensor_scan(out=r_im[:, :n], data0=Mm, data1=rin_im[:, :n], initial=init[:, gpl, 1:2], op0=ALU.mult, op1=ALU.add),
                               [kM, "rin_im", ki], ["r_im"])
                        if wi < len(order) - 1:
                            e1 = n - 1
                            dv(lambda e, gpl=gpl, e1=e1: e.tensor_tensor(out=init[:, gpl, 2:3], in0=r_re[:, e1:e1 + 1], in1=tabC[gpl][:, e1:e1 + 1], op=ALU.mult), ["r_re", kC, ki], [ki])
                            dv(lambda e, gpl=gpl, e1=e1: e.tensor_tensor(out=init[:, gpl, 3:4], in0=r_im[:, e1:e1 + 1], in1=tabS[gpl][:, e1:e1 + 1], op=ALU.mult), ["r_im", kS, ki], [ki])
                            dv(lambda e, gpl=gpl, e1=e1: e.tensor_tensor(out=init[:, gpl, 4:5], in0=r_re[:, e1:e1 + 1], in1=tabS[gpl][:, e1:e1 + 1], op=ALU.mult), ["r_re", kS, ki], [ki])
                            dv(lambda e, gpl=gpl, e1=e1: e.tensor_tensor(out=init[:, gpl, 5:6], in0=r_im[:, e1:e1 + 1], in1=tabC[gpl][:, e1:e1 + 1], op=ALU.mult), ["r_im", kC, ki], [ki])
                            dv(lambda e, gpl=gpl: e.tensor_tensor(out=init[:, gpl, 0:1], in0=init[:, gpl, 2:3], in1=init[:, gpl, 3:4], op=ALU.subtract), [ki], [ki])
                            dv(lambda e, gpl=gpl: e.tensor_tensor(out=init[:, gpl, 1:2], in0=init[:, gpl, 4:5], in1=init[:, gpl, 5:6], op=ALU.add), [ki], [ki])
                        K.op("pool", lambda e, n=n, Cc=Cc: e.tensor_tensor(out=u1[:, :n], in0=r_re[:, :n], in1=Cc, op=ALU.mult), reads=["r_re", kC], writes=["q1"])
                        K.op("pool", lambda e, n=n, Ss=Ss: e.tensor_tensor(out=u2[:, :n], in0=r_im[:, :n], in1=Ss, op=ALU.mult), reads=["r_im", kS], writes=["q2"])
                        dv(lambda e, n=n, Ss=Ss: e.tensor_tensor(out=u3[:, :n], in0=r_re[:, :n], in1=Ss, op=ALU.mult), ["r_re", kS], ["q3"])
                        dv(lambda e, n=n, Cc=Cc: e.tensor_tensor(out=u4[:, :n], in0=r_im[:, :n], in1=Cc, op=ALU.mult), ["r_im", kC], ["q4"])
                        a1 = u1[:, :n]; a2 = u2[:, :n]; a3 = u3[:, :n]; a4 = u4[:, :n]
                        if rev:
                            a1 = a1[:, ::-1]; a2 = a2[:, ::-1]; a3 = a3[:, ::-1]; a4 = a4[:, ::-1]
                        K.op("pool", lambda e, n=n, a1=a1, a2=a2, b2=b2: e.tensor_tensor(out=s_re[b2][:, :n], in0=a1, in1=a2, op=ALU.subtract),
                             reads=["q1", "q2"], writes=[("s_re", b2)])
                        dv(lambda e, n=n, a3=a3, a4=a4, b2=b2: e.tensor_tensor(out=s_im[b2][:, :n], in0=a3, in1=a4, op=ALU.add),
                           ["q3", "q4"], [("s_im", b2)])
                        K.op("pe", lambda e, n=n, gpl=gpl, b2=b2, yb=yb: e.matmul(ps[yb][:, :n], lhsT=lhs[gpl][2], rhs=s_re[b2][:, :n],
                                                                               start=(gpl == 0), stop=False),
                             reads=[("lhs", gpl, 2), ("s_re", b2)], writes=[PS[yb]])
                        K.op("pe", lambda e, n=n, gpl=gpl, b2=b2, yb=yb: e.matmul(ps[yb][:, :n], lhsT=lhs[gpl][3], rhs=s_im[b2][:, :n],
                                                                               start=False, stop=(gpl == 3)),
                             reads=[("lhs", gpl, 3), ("s_im", b2)], writes=[PS[yb]])
                    ky = ("yacc", ti)
                    if r == 0:
                        K.op("act", lambda e, yb=yb, t0=t0, n=n: e.activation(out=yacc[:, t0:t0 + n], in_=ps[yb][:, :n], func=AF.Copy),
                             reads=[PS[yb]], writes=[ky])
                    else:
                        dv(lambda e, yb=yb, t0=t0, n=n: e.tensor_tensor(out=yacc[:, t0:t0 + n], in0=ps[yb][:, :n], in1=yacc[:, t0:t0 + n], op=ALU.add),
                           [PS[yb], ky], [ky])
                        dv(lambda e, t0=t0, n=n: e.scalar_tensor_tensor(out=yacc[:, t0:t0 + n], in0=hT[:, cc, t0:t0 + n], scalar=dvec[:, cc:cc + 1],
                                                                       in1=yacc[:, t0:t0 + n], op0=ALU.mult, op1=ALU.add),
                           [("hT", ti), ky, "dvec"], [ky])
                        K.op("act", lambda e, t0=t0, n=n: e.activation(out=hT[:, cc, t0:t0 + n], in_=yacc[:, t0:t0 + n], func=AF.Gelu),
                             reads=[ky], writes=[("hT", ti)])
        K.fence()
        al = Al()
        xt = [al.f(4096).rearrange("p (c t) -> p c t", c=NCH) for i in range(2)]
        sg = [al.f(512) for i in range(2)]
        oo = [al.f(512) for i in range(2)]
        glub2 = al.f(16)
        wglu = al.b(NCH * 2048).rearrange("p (k o) -> p k o", k=NCH)
        K.dma("sp", glub2, dr["ssm_glu_b"][j].rearrange("(c p) -> p c", p=128), writes=["glub2"], allow_slow_non_contiguous=True)
        K.dma("pool", wglu, dr["ssm_glu_w"][j].rearrange("(k p) o -> p k o", p=128), writes=["wglu"])
        for ti, (t0, n, col) in enumerate(TILES):
            b2 = ti % 2
            kx = ("xt", b2)
            K.dma(dmaq(ti), xt[b2][:, :, :n], xT[:, :, t0:t0 + n].rearrange("c p t -> p c t"), writes=[kx, kx + ("n",)])
            for c in range(NCH):
                c2 = c % 2
                pa, pg = 2 * c2, 2 * c2 + 1
                for k in range(NCH):
                    K.op("pe", lambda e, pa=pa, c=c, k=k, t0=t0, n=n: e.matmul(ps[pa][:, :n], lhsT=wglu[:, k, c * 128:(c + 1) * 128],
                                                                             rhs=hT[:, k, t0:t0 + n], start=(k == 0), stop=(k == NCH - 1)),
                         reads=["wglu", ("hT", ti)], writes=[PS[pa]])
                for k in range(NCH):
                    K.op("pe", lambda e, pg=pg, c=c, k=k, t0=t0, n=n: e.matmul(ps[pg][:, :n], lhsT=wglu[:, k, 1024 + c * 128:1024 + (c + 1) * 128],
                                                                             rhs=hT[:, k, t0:t0 + n], start=(k == 0), stop=(k == NCH - 1)),
                         reads=["wglu", ("hT", ti)], writes=[PS[pg]])
                K.op("act", lambda e, pg=pg, c=c, c2=c2, n=n: e.activation(out=sg[c2][:, :n], in_=ps[pg][:, :n], func=AF.Sigmoid,
                                                                          bias=glub2[:, 8 + c:9 + c], scale=1.0),
                     reads=[PS[pg], "glub2"], writes=[("sg", c2)])
                K.op("dve", lambda e, pa=pa, c=c, c2=c2, n=n: e.scalar_tensor_tensor(out=oo[c2][:, :n], in0=ps[pa][:, :n], scalar=glub2[:, c:c + 1],
                                                                                    in1=sg[c2][:, :n], op0=ALU.add, op1=ALU.mult),
                     reads=[PS[pa], ("sg", c2), "glub2"], writes=[("oo", c2)])
                K.op("dve", lambda e, c=c, c2=c2, n=n, col=col, b2=b2: e.scalar_tensor_tensor(
                    out=xt[b2][:, c, :n], in0=oo[c2][:, :n], scalar=modv[:, l, 2, c, col:col + 1], in1=xt[b2][:, c, :n],
                    op0=ALU.mult, op1=ALU.add), reads=[("oo", c2), kx], writes=[kx + ("n",)])
            K.dma(dmaq(ti + 1), xT[:, :, t0:t0 + n].rearrange("c p t -> p c t"), xt[b2][:, :, :n], reads=[kx, kx + ("n",)], writes=[("xTw", ti)])
        K.fence()

    def attn_phase(l, need_ctx):
        j = l // 2
        for hp in range(2):
            attn_half(l, j, hp, need_ctx)

    def attn_half(l, j, hp, need_ctx):
        al = Al()
        fa, fb = al.f, al.b
        KT = fb(2 * T).rearrange("p (h t) -> p h t", h=2)
        Vs = fb(34 * 128).rearrange("p (s d) -> p s d", s=34)
        qh = [fb(512) for _ in range(2)]
        pT = [fb(512) for _ in range(3)]
        sqb = [fb(512) for _ in range(2)]
        qnb = [fb(512) for _ in range(2)]
        Osb = fb(8 * 512).rearrange("p (h t) -> p h t", h=8)
        rotb = fb(64); onesq = fb(64)
        wq = fb(NCH * 768).rearrange("p (k o) -> p k o", k=NCH)
        wo = fb(8 * 1024).rearrange("p (h o) -> p h o", h=8)
        xc = [fa(512) for _ in range(2)]
        cosT = [fa(512) for _ in range(2)]; sinT = [fa(512) for _ in range(2)]
        ms = fa(512); rinv = fa(512); rstd = fa(512); qn = fa(512); t1 = fa(512); t2 = fa(512)
        rden = [fa(512) for _ in range(2)]
        gq = fa(8); rot_f = fa(64)

        wsrc = dr["attn_w_qkv"][j]
        K.dma("pool", wq[:, :, 0:512], wsrc[:, hp * 512:(hp + 1) * 512].rearrange("(k p) o -> p k o", p=128), writes=["wq"])
        K.dma("pool", wq[:, :, 512:640], wsrc[:, 1024 + hp * 128:1024 + (hp + 1) * 128].rearrange("(k p) o -> p k o", p=128), writes=["wq"])
        K.dma("pool", wq[:, :, 640:768], wsrc[:, 1280 + hp * 128:1280 + (hp + 1) * 128].rearrange("(k p) o -> p k o", p=128), writes=["wq"])
        K.dma("pool", wo[0:64, :, :], dr["attn_w_o"][j][hp * 512:(hp + 1) * 512, :].rearrange("(h d) o -> d h o", d=64), writes=["wo"])
        K.dma("sp", rot_f[0:64, :], dr["k_rotT"][:, :], writes=["rot_f"])
        K.op("dve", lambda e: e.tensor_copy(out=rotb[0:64, :], in_=rot_f[0:64, :]), reads=["rot_f"], writes=["rotb"])
        K.op("dve", lambda e: e.memset(onesq[0:64, :], 1.0), writes=["onesq"])
        K.dma("sp", gq[0:64, 0:1], dr["attn_q_g"][j].rearrange("(d o) -> d o", o=1), writes=["gq"], allow_slow_non_contiguous=True)
        K.dma("sp", gq[0:64, 1:2], dr["attn_k_g"][j].rearrange("(d o) -> d o", o=1), writes=["gq"], allow_slow_non_contiguous=True)
        K.op("dve", lambda e: e.tensor_scalar(out=gq[0:64, 2:3], in0=gq[0:64, 0:1], scalar1=0.125, scalar2=None, op0=ALU.mult),
             reads=["gq"], writes=["gq2"])

        cnt = [0]

        def proj_head(wcol, gcol, gkey, ti, t0, n, rope, dst, kdst):
            i = cnt[0] % 2
            cnt[0] += 1
            pq, pn = 2, 3
            for k in range(NCH):
                K.op("pe", lambda e, k=k: e.matmul(ps[pq][0:64, :n], lhsT=wq[:, k, wcol:wcol + 64], rhs=hT[:, k, t0:t0 + n],
                                                   start=(k == 0), stop=(k == NCH - 1)), reads=["wq", ("hT", ti)], writes=[PS[pq]])
            K.op("act", lambda e: e.activation(out=sqb[i][0:64, :n], in_=ps[pq][0:64, :n], func=AF.Square), reads=[PS[pq]], writes=[("sqb", i)])
            K.op("pe", lambda e: e.matmul(ps[pn][0:64, :n], lhsT=onesq[0:64, :], rhs=sqb[i][0:64, :n], start=True, stop=True),
                 reads=[("sqb", i), "onesq"], writes=[PS[pn]])
            K.op("dve", lambda e: e.tensor_scalar(out=ms[0:64, :n], in0=ps[pn][0:64, :n], scalar1=1.0 / 64, scalar2=EPS, op0=ALU.mult, op1=ALU.add),
                 reads=[PS[pn]], writes=["ms"])
            K.op("dve", lambda e: e.reciprocal(out=rinv[0:64, :n], in_=ms[0:64, :n]), reads=["ms"], writes=["rinv"])
            K.op("act", lambda e: e.activation(out=rstd[0:64, :n], in_=rinv[0:64, :n], func=AF.Sqrt), reads=["rinv"], writes=["rstd"])
            if not rope:
                K.op("dve", lambda e: e.scalar_tensor_tensor(out=dst, in0=ps[pq][0:64, :n], scalar=gq[0:64, gcol:gcol + 1], in1=rstd[0:64, :n],
                                                             op0=ALU.mult, op1=ALU.mult), reads=[PS[pq], "rstd", gkey], writes=[kdst])
                return
            K.op("dve", lambda e: e.scalar_tensor_tensor(out=qn[0:64, :n], in0=ps[pq][0:64, :n], scalar=gq[0:64, gcol:gcol + 1], in1=rstd[0:64, :n],
                                                         op0=ALU.mult, op1=ALU.mult), reads=[PS[pq], "rstd", gkey], writes=["qn"])
            K.op("act", lambda e: e.activation(out=qnb[i][0:64, :n], in_=qn[0:64, :n], func=AF.Copy), reads=["qn"], writes=[("qnb", i)])
            K.op("pe", lambda e: e.matmul(ps[pn][0:64, :n], lhsT=rotb[0:64, :], rhs=qnb[i][0:64, :n], start=True, stop=True),
                 reads=[("qnb", i), "rotb"], writes=[PS[pn]])
            tb = ti % 2
            K.op("pool", lambda e: e.tensor_tensor(out=t1[0:64, :n], in0=qn[0:64, :n], in1=cosT[tb][0:64, :n], op=ALU.mult),
                 reads=["qn", ("rope", tb)], writes=["t1"])
            K.op("dve", lambda e: e.tensor_tensor(out=t2[0:64, :n], in0=ps[pn][0:64, :n], in1=sinT[tb][0:64, :n], op=ALU.mult),
                 reads=[PS[pn], ("rope", tb)], writes=["t2"])
            K.op("dve", lambda e: e.tensor_tensor(out=dst, in0=t1[0:64, :n], in1=t2[0:64, :n], op=ALU.add), reads=["t1", "t2"], writes=[kdst])

        def load_rope(ti, t0, n):
            tb = ti % 2
            p0 = t0 - T_CTX
            K.dma("sp", cosT[tb][0:64, :n], dr["k_cosF"][:, p0:p0 + n], writes=[("rope", tb)])
            K.dma("act", sinT[tb][0:64, :n], dr["k_sinF"][:, p0:p0 + n], writes=[("rope", tb)])

        for ti, (t0, n, col) in enumerate(TILES):
            rope = col == 0
            if rope:
                load_rope(ti, t0, n)
            for khl in range(2):
                proj_head(512 + khl * 64, 1, "gq", ti, t0, n, rope, KT[0:64, khl, t0:t0 + n], ("KT", ti))
            for s_ in range(n // 128):
                sc = t0 // 128 + s_
                pv = 4 + sc % 2
                for k in range(NCH):
                    K.op("pe", lambda e, k=k, sc=sc, pv=pv: e.matmul(ps[pv][:, 0:128], lhsT=hT[:, k, sc * 128:(sc + 1) * 128], rhs=wq[:, k, 640:768],
                                                                   start=(k == 0), stop=(k == NCH - 1)), reads=["wq", ("hT", ti)], writes=[PS[pv]])
                K.op("act", lambda e, sc=sc, pv=pv: e.activation(out=Vs[:, sc, :], in_=ps[pv][:, 0:128], func=AF.Copy), reads=[PS[pv]], writes=[("V", ti)])
        for ti, (t0, n, col) in enumerate(TILES):
            if col == 1 and not need_ctx:
                continue
            rope = col == 0
            if rope:
                load_rope(ti, t0, n)
            s_chunks = list(range(2)) if col == 1 else list(range(34))
            for hl in range(8):
                khl = hl // 4
                hb = hl % 2
                proj_head(hl * 64, 2, "gq2", ti, t0, n, rope, qh[hb][0:64, :n], ("qh", hb))
                po, pd = 4 + hb, 6 + hb
                for si, sc in enumerate(s_chunks):
                    pS = si % 2
                    p3 = si % 3
                    kt_ti = 0 if sc < 2 else 1 + (sc - 2) // 4
                    K.op("pe", lambda e: e.matmul(ps[pS][:, :n], lhsT=KT[0:64, khl, sc * 128:(sc + 1) * 128], rhs=qh[hb][0:64, :n],
                                                  start=True, stop=True), reads=[("KT", kt_ti), ("qh", hb)], writes=[PS[pS]])
                    K.op("act", lambda e: e.activation(out=pT[p3][:, :n], in_=ps[pS][:, :n], func=AF.Exp), reads=[PS[pS]], writes=[("pT", p3)])
                    K.op("pe", lambda e: e.matmul(ps[po][0:64, :n], lhsT=Vs[:, sc, khl * 64:(khl + 1) * 64], rhs=pT[p3][:, :n],
                                                  start=(si == 0), stop=(si == len(s_chunks) - 1)),
                         reads=[("V", kt_ti), ("pT", p3)], writes=[PS[po]])
                    K.op("pe", lambda e: e.matmul(ps[pd][0:64, :n], lhsT=ones_b[:, 0:64], rhs=pT[p3][:, :n],
                                                  start=(si == 0), stop=(si == len(s_chunks) - 1)),
                         reads=["ones_b", ("pT", p3)], writes=[PS[pd]])
                K.op("dve", lambda e: e.reciprocal(out=rden[hb][0:64, :n], in_=ps[pd][0:64, :n]), reads=[PS[pd]], writes=[("rden", hb)])
                K.op("dve", lambda e: e.tensor_tensor(out=Osb[0:64, hl, :n], in0=ps[po][0:64, :n], in1=rden[hb][0:64, :n], op=ALU.mult),
                     reads=[PS[po], ("rden", hb)], writes=[("Osb", hl)])
            for c in range(NCH):
                pw = 2 + c % 2
                xb2 = c % 2
                kx = ("xc", xb2)
                K.dma(dmaq(c), xc[xb2][:, :n], xT[c, :, t0:t0 + n], reads=[("xTw", ti, c)], writes=[kx])
                for hl in range(8):
                    K.op("pe", lambda e: e.matmul(ps[pw][:, :n], lhsT=wo[0:64, hl, c * 128:(c + 1) * 128], rhs=Osb[0:64, hl, :n],
                                                  start=(hl == 0), stop=(hl == 7)), reads=["wo", ("Osb", hl)], writes=[PS[pw]])
                K.op("dve", lambda e: e.scalar_tensor_tensor(
                    out=xc[xb2][:, :n], in0=ps[pw][:, :n], scalar=modv[:, l, 2, c, col:col + 1], in1=xc[xb2][:, :n],
                    op0=ALU.mult, op1=ALU.add), reads=[PS[pw], kx], writes=[kx])
                K.dma(dmaq(c + 1), xT[c, :, t0:t0 + n], xc[xb2][:, :n], reads=[kx], writes=[("xTw", ti, c)])
        K.fence()

    def ffn_phase(l, moe, need_ctx):
        j = l // 2
        n_exp = NE if moe else 1
        tiles = [(ti,) + TILES[ti] for ti in range(len(TILES)) if need_ctx or TILES[ti][2] == 0]
        passes = [tiles[i:i + 3] for i in range(0, len(tiles), 3)]
        al = Al()
        yacc = al.f(12288).rearrange("p (c t) -> p c t", c=NCH)
        sgt = [al.f(512) for _ in range(2)]
        lg = al.f(8); m8 = al.f(8); ex = al.f(8); mk = al.f(8); gs = al.f(8); nt1 = al.f(8); rgs = al.f(8); gt = al.f(8); rb = al.f(8)
        wblk = [al.b(12288) for _ in range(2)]
        gatesT = al.b(T)
        esel = al.b(NE * 128).rearrange("p (e m) -> p e m", e=NE)
        act_t = [al.b(2048).rearrange("p (f t) -> p f t", f=4) for _ in range(2)]
        act_u = [al.b(512) for _ in range(2)]
        gbc = al.b(1536)
        wr = al.b(NCH * NE).rearrange("p (k e) -> p k e", k=NCH)

        if moe:
            K.dma("pool", wr, dr["moe_router_w"][j].rearrange("(k p) e -> p k e", p=128), writes=["wr"])
            rb_src = bass.AP(dr["moe_router_b"].tensor, j * NE, [[0, 128], [1, NE]])
            K.dma("sp", rb, rb_src, writes=["rb"])
            for ee in range(NE):
                K.op("dve", lambda e: e.tensor_scalar(out=esel[0:8, ee, :], in0=ones_f[0:8, 0:128], scalar1=ident[0:8, ee:ee + 1], scalar2=None,
                                                      op0=ALU.mult), reads=["ident", "ones_f"], writes=["esel"])
            for sc in range(T // 128):
                ti = 0 if sc < 2 else 1 + (sc - 2) // 4
                if TILES[ti][2] == 1 and not need_ctx:
                    continue
                pb = sc % 2
                for k in range(NCH):
                    K.op("pe", lambda e: e.matmul(ps[pb][:, 0:NE], lhsT=hT[:, k, sc * 128:(sc + 1) * 128], rhs=wr[:, k, :],
                                                  start=(k == 0), stop=(k == NCH - 1)), reads=["wr", ("hT", ti)], writes=[PS[pb]])
                K.op("dve", lambda e: e.tensor_tensor(out=lg, in0=ps[pb][:, 0:NE], in1=rb, op=ALU.add), reads=[PS[pb], "rb"], writes=["lg"])
                K.op("dve", lambda e: e.max(out=m8, in_=lg), reads=["lg"], writes=["m8"])
                K.op("dve", lambda e: e.tensor_scalar(out=nt1[:, 0:1], in0=m8[:, 0:1], scalar1=-1.0, scalar2=None, op0=ALU.mult), reads=["m8"], writes=["nt1"])
                K.op("act", lambda e: e.activation(out=ex, in_=lg, func=AF.Exp, bias=nt1[:, 0:1], scale=1.0), reads=["lg", "nt1"], writes=["ex"])
                K.op("dve", lambda e: e.tensor_scalar(out=mk, in0=lg, scalar1=m8[:, 1:2], scalar2=None, op0=ALU.is_ge), reads=["lg", "m8"], writes=["mk"])
                K.op("dve", lambda e: e.tensor_tensor(out=ex, in0=ex, in1=mk, op=ALU.mult), reads=["ex", "mk"], writes=["ex"])
                K.op("dve", lambda e: e.tensor_reduce(out=gs[:, 0:1], in_=ex, axis=mybir.AxisListType.X, op=ALU.add), reads=["ex"], writes=["gs"])
                K.op("dve", lambda e: e.reciprocal(out=rgs[:, 0:1], in_=gs[:, 0:1]), reads=["gs"], writes=["rgs"])
                K.op("dve", lambda e: e.tensor_scalar(out=gt, in0=ex, scalar1=rgs[:, 0:1], scalar2=None, op0=ALU.mult), reads=["ex", "rgs"], writes=["gt"])
                K.op("pe", lambda e: e.transpose(ps[2 + pb][0:8, 0:128], gt, ident[:]), reads=["gt", "ident"], writes=[PS[2 + pb]])
                K.op("act", lambda e: e.activation(out=gatesT[0:8, sc * 128:(sc + 1) * 128], in_=ps[2 + pb][0:8, 0:128], func=AF.Copy),
                     reads=[PS[2 + pb]], writes=[("gatesT", ti)])

        blocks = [(0, 4), (4, 4), (8, 4), (12, 4), (16, 3), (19, 3)]
        wcount = [0]
        for pss in passes:
            offs = []
            a_ = 0
            for t_ in pss:
                offs.append(a_)
                a_ += t_[2]
            first = True
            for ee in range(n_exp):
                if moe:
                    wgu = dr["moe_w_gate_up"][j, ee]; wdn = dr["moe_w_down"][j, ee]
                    for pi, (ti, t0, n, col) in enumerate(pss):
                        K.op("pe", lambda e: e.matmul(ps[6][:, :n], lhsT=esel[0:8, ee, :], rhs=gatesT[0:8, t0:t0 + n], start=True, stop=True),
                             reads=["esel", ("gatesT", ti)], writes=[PS[6]])
                        K.op("act", lambda e: e.activation(out=gbc[:, offs[pi]:offs[pi] + n], in_=ps[6][:, :n], func=AF.Copy),
                             reads=[PS[6]], writes=[("gbc", pi)])
                else:
                    wgu = dr["ffn_w_gate_up"][j]; wdn = dr["ffn_w_down"][j]
                for (f0, nf) in blocks:
                    wb = wcount[0] % 2
                    wcount[0] += 1
                    W = wblk[wb]
                    Wg = W[:, 0:NCH * 512].rearrange("p (k o) -> p k o", k=NCH)
                    Wu = W[:, 4096:4096 + NCH * 512].rearrange("p (k o) -> p k o", k=NCH)
                    Wd = W[:, 8192:12288].rearrange("p (f o) -> p f o", f=4)
                    kw = ("W", wb)
                    K.dma("pool", Wg[:, :, :nf * 128], wgu[:, f0 * 128:(f0 + nf) * 128].rearrange("(k p) o -> p k o", p=128), writes=[kw])
                    K.dma("pool", Wu[:, :, :nf * 128], wgu[:, DFF + f0 * 128:DFF + (f0 + nf) * 128].rearrange("(k p) o -> p k o", p=128), writes=[kw])
                    K.dma("pool", Wd[:, :nf, :], wdn[f0 * 128:(f0 + nf) * 128, :].rearrange("(f p) o -> p f o", p=128), writes=[kw])
                    for pi, (ti, t0, n, col) in enumerate(pss):
                        ab = pi % 2
                        o_ = offs[pi]
                        for fi in range(nf):
                            f2 = fi % 2
                            pg, pu = 0 + f2, 2 + f2
                            for k in range(NCH):
                                K.op("pe", lambda e: e.matmul(
                                    ps[pg][:, :n], lhsT=Wg[:, k, fi * 128:(fi + 1) * 128], rhs=hT[:, k, t0:t0 + n], start=(k == 0), stop=(k == NCH - 1)),
                                    reads=[kw, ("hT", ti)], writes=[PS[pg]])
                            for k in range(NCH):
                                K.op("pe", lambda e: e.matmul(
                                    ps[pu][:, :n], lhsT=Wu[:, k, fi * 128:(fi + 1) * 128], rhs=hT[:, k, t0:t0 + n], start=(k == 0), stop=(k == NCH - 1)),
                                    reads=[kw, ("hT", ti)], writes=[PS[pu]])
                            K.op("act", lambda e: e.activation(out=sgt[f2][:, :n], in_=ps[pg][:, :n], func=AF.Silu),
                                 reads=[PS[pg]], writes=[("sgt", f2)])
                            if moe:
                                K.op("dve", lambda e: e.tensor_tensor(out=act_u[f2][:, :n], in0=sgt[f2][:, :n], in1=ps[pu][:, :n], op=ALU.mult),
                                     reads=[("sgt", f2), PS[pu]], writes=[("actu", f2)])
                                K.op("pool", lambda e: e.tensor_tensor(
                                    out=act_t[ab][:, fi, :n], in0=act_u[f2][:, :n], in1=gbc[:, o_:o_ + n], op=ALU.mult),
                                    reads=[("actu", f2), ("gbc", pi)], writes=[("act", ab, fi)])
                            else:
                                K.op("dve", lambda e: e.tensor_tensor(
                                    out=act_t[ab][:, fi, :n], in0=sgt[f2][:, :n], in1=ps[pu][:, :n], op=ALU.mult),
                                    reads=[("sgt", f2), PS[pu]], writes=[("act", ab, fi)])
                        for c in range(NCH):
                            py = 4 + c % 2
                            for fi in range(nf):
                                K.op("pe", lambda e: e.matmul(
                                    ps[py][:, :n], lhsT=Wd[:, fi, c * 128:(c + 1) * 128], rhs=act_t[ab][:, fi, :n], start=(fi == 0), stop=(fi == nf - 1)),
                                    reads=[kw, ("act", ab, fi)], writes=[PS[py]])
                            ky = ("yacc", pi, c)
                            if first:
                                K.op("act", lambda e: e.activation(out=yacc[:, c, o_:o_ + n], in_=ps[py][:, :n], func=AF.Copy),
                                     reads=[PS[py]], writes=[ky])
                            else:
                                K.op("dve", lambda e: e.tensor_tensor(out=yacc[:, c, o_:o_ + n], in0=ps[py][:, :n],
                                                                      in1=yacc[:, c, o_:o_ + n], op=ALU.add),
                                     reads=[PS[py], ky], writes=[ky])
                    first = False
            for pi, (ti, t0, n, col) in enumerate(pss):
                o_ = offs[pi]
                for c in range(NCH):
                    xb2 = c % 2
                    kx = ("xtf", xb2)
                    K.dma(dmaq(c), sgt[xb2][:, :n], xT[c, :, t0:t0 + n], writes=[kx, ("sgt", xb2)])
                    K.op("dve", lambda e: e.scalar_tensor_tensor(
                        out=sgt[xb2][:, :n], in0=yacc[:, c, o_:o_ + n], scalar=modv[:, l, 5, c, col:col + 1], in1=sgt[xb2][:, :n],
                        op0=ALU.mult, op1=ALU.add), reads=[("yacc", pi, c), kx], writes=[kx, ("sgt", xb2)])
                    K.dma(dmaq(c + 1), xT[c, :, t0:t0 + n], sgt[xb2][:, :n], reads=[kx, ("sgt", xb2)], writes=[("xTw", ti, c)])
            K.fence()

    phase = [0]

    def done():
        phase[0] += 1
        return stop_after is not None and phase[0] >= stop_after

    stopped = False
    for l in range(4):
        last = l == 3
        norm_phase(l, Amix, 0)
        if l % 2 == 0:
            s5_phase(l)
        else:
            attn_phase(l, need_ctx=not last)
        if done():
            stopped = True
            break
        norm_phase(l, Affn, 3)
        ffn_phase(l, moe=(l % 2 == 1), need_ctx=not last)
        if done():
            stopped = True
            break

    al = Al()
    xi = [al.f(1024).rearrange("p (c t) -> p c t", c=NCH) for i in range(2)]
    xo2 = [al.f(1024) for i in range(2)]
    for blk in range(T_LAT // 128):
        b2 = blk % 2
        t0 = T_CTX + blk * 128
        K.dma(dmaq(blk), xi[b2], xT[:, :, t0:t0 + 128].rearrange("c p t -> p c t"), writes=[("xi", b2)])
        for half in range(2):
            bank = 2 * b2 + half
            for jj in range(4):
                c = half * 4 + jj
                K.op("pe", lambda e, bank=bank, jj=jj, c=c, b2=b2: e.transpose(ps[bank][:, jj * 128:(jj + 1) * 128], xi[b2][:, c, :], ident[:]),
                     reads=[("xi", b2), "ident"], writes=[PS[bank]])
            if half == 0:
                K.op("act", lambda e, bank=bank, b2=b2: e.activation(out=xo2[b2][:, 0:512], in_=ps[bank][:], func=AF.Copy),
                     reads=[PS[bank]], writes=[("xo2", b2, 0)])
            else:
                K.op("dve", lambda e, bank=bank, b2=b2: e.tensor_copy(out=xo2[b2][:, 512:1024], in_=ps[bank][:]),
                     reads=[PS[bank]], writes=[("xo2", b2, 1)])
        K.dma(dmaq(blk + 1), out_d[blk * 128:(blk + 1) * 128, :], xo2[b2], reads=[("xo2", b2, 0), ("xo2", b2, 1)], final=True)
    counts = K.emit()
    st.close()
    return nc, counts


_CACHE = {}


def kernel(**inputs):
    stop_after = inputs.pop("_stop_after", None)
    n_cores = 4
    key = stop_after
    if key not in _CACHE:
        _CACHE[key] = build(stop_after)
    nc, counts = _CACHE[key]
    consts = _consts()
    in_maps = []
    for b in range(n_cores):
        m = {}
        for k, shp in IN_SHAPES.items():
            if k in consts:
                m[k] = consts[k]
            elif k == "x":
                m[k] = np.ascontiguousarray(inputs["x"][b], dtype=np.float32)
            elif k == "ctx":
                m[k] = np.ascontiguousarray(inputs["ctx"][b], dtype=np.float32)
            elif k == "c":
                m[k] = np.ascontiguousarray(inputs["c"][b:b + 1], dtype=np.float32)
            elif k == "c_ctx":
                m[k] = np.ascontiguousarray(np.asarray(inputs["c_ctx"], dtype=np.float32).reshape(1, D))
            else:
                m[k] = np.ascontiguousarray(inputs[k], dtype=np.float32)
        in_maps.append(m)
    res = run_bass_kernel_spmd(nc, in_maps, core_ids=list(range(n_cores)))
    out = np.stack([np.asarray(res.results[b]["out"], dtype=np.float32) for b in range(n_cores)], axis=0)
    return out
```

```python
import contextlib
import numpy as np
import concourse.bass as bass
import concourse.mybir as mybir
from concourse.bass_utils import run_bass_kernel_spmd

F32 = mybir.dt.float32
BF16 = mybir.dt.bfloat16
I32 = mybir.dt.int32
AF = mybir.ActivationFunctionType
ALU = mybir.AluOpType

ENGS = ("pe", "act", "dve", "pool", "sp")
N_DMA_SEMS = 40

D = 1024
NCH = 8
T_CTX = 256
T_LAT = 4096
T = T_CTX + T_LAT
DFF = 2816
NFC = 22
NE = 8
EPS = 1e-6
TILES = [(0, 256, 1)] + [(256 + 512 * i, 512, 0) for i in range(8)]
TWO_PI = float(2 * np.pi)


class Emitter:
    def __init__(self, nc):
        self.nc = nc
        self.ins = {e: [] for e in ENGS}
        self.lastw = {}
        self.readers = {}
        self.clock = {e: {} for e in ENGS}
        self.dma_seen = {e: set() for e in ENGS}
        self.n_dma = 0
        self.dma_sem_last = [None] * N_DMA_SEMS
        self.dma_sem_cnt = [0] * N_DMA_SEMS
        self.final_dma = []

    def _need(self, eng, ev, waits):
        if ev is None:
            return
        if ev[0] == "c":
            _, f, idx = ev
            if f == eng and eng == "pe":
                return
            if self.clock[eng].get(f, -1) >= idx:
                return
            waits.append(ev)
        else:
            if ev[1] in self.dma_seen[eng]:
                return
            waits.append(ev)

    def _apply_waits(self, eng, waits):
        best = {}
        dm = {}
        for ev in waits:
            if ev[0] == "c":
                best[ev[1]] = max(best.get(ev[1], -1), ev[2])
            else:
                dm[ev[1]] = ev
        out = []
        for f, idx in best.items():
            out.append(("c", f, idx))
            self.ins[f][idx]["marked"] = True
            src = self.ins[f][idx]["clk"]
            for g, v in src.items():
                if self.clock[eng].get(g, -1) < v:
                    self.clock[eng][g] = v
            if self.clock[eng].get(f, -1) < idx:
                self.clock[eng][f] = idx
        for did, ev in dm.items():
            out.append(ev)
            self.dma_seen[eng].add(did)
        return out

    def _deps(self, eng, reads, writes):
        waits = []
        for k in reads:
            self._need(eng, self.lastw.get(k), waits)
        for k in writes:
            self._need(eng, self.lastw.get(k), waits)
            for ev in self.readers.get(k, ()):
                self._need(eng, ev, waits)
        return self._apply_waits(eng, waits)

    def _commit(self, ev, reads, writes):
        for k in reads:
            self.readers.setdefault(k, []).append(ev)
        for k in writes:
            self.lastw[k] = ev
            self.readers[k] = []

    def op(self, eng, fn, reads=(), writes=()):
        waits = self._deps(eng, reads, writes)
        idx = len(self.ins[eng])
        calls = []

        class _Rec:
            def __getattr__(self_, name):
                return lambda *a, **k: calls.append((name, a, k))
        fn(_Rec())
        assert len(calls) == 1, calls
        name, a, k = calls[0]
        fn = (lambda e, name=name, a=a, k=k: getattr(e, name)(*a, **k))
        self.ins[eng].append({"fn": fn, "waits": waits, "marked": False, "dma": None,
                              "clk": dict(self.clock[eng])})
        self._commit(("c", eng, idx), reads, writes)

    def dma(self, eng, out, in_, reads=(), writes=(), final=False, **kw):
        s = self.n_dma % N_DMA_SEMS
        waits = self._deps(eng, reads, writes)
        prev = self.dma_sem_last[s]
        if prev is not None and prev[1] not in self.dma_seen[eng]:
            waits.append(prev)
            self.dma_seen[eng].add(prev[1])
        self.dma_sem_cnt[s] += 1
        ev = ("d", self.n_dma, s, 16 * self.dma_sem_cnt[s])
        self.dma_sem_last[s] = ev
        self.n_dma += 1
        self.ins[eng].append({"fn": (lambda e, out=out, in_=in_, kw=kw: e.dma_start(out=out, in_=in_, **kw)),
                              "waits": waits, "marked": False, "dma": ev, "clk": dict(self.clock[eng])})
        self._commit(ev, reads, writes)
        if final:
            self.final_dma.append(ev)

    def cc(self, kind, groups, in_ap, out_ap, reads=(), writes=()):
        eng = "pool"
        s = self.n_dma % N_DMA_SEMS
        waits = self._deps(eng, reads, writes)
        prev = self.dma_sem_last[s]
        if prev is not None and prev[1] not in self.dma_seen[eng]:
            waits.append(prev)
            self.dma_seen[eng].add(prev[1])
        self.dma_sem_cnt[s] += 1
        ev = ("d", self.n_dma, s, 16 * self.dma_sem_cnt[s])
        self.dma_sem_last[s] = ev
        self.n_dma += 1
        self.ins[eng].append({"fn": (lambda e: e.collective_compute(kind, ALU.bypass, replica_groups=groups, ins=[in_ap], outs=[out_ap])),
                              "waits": waits, "marked": False, "dma": ev, "clk": dict(self.clock[eng])})
        self._commit(ev, reads, writes)

    def fence(self):
        last = {}
        for e in ENGS:
            idx = len(self.ins[e]) - 1
            while idx >= 0 and (self.ins[e][idx]["dma"] is not None or self.ins[e][idx]["fn"] is None):
                idx -= 1
            if idx >= 0:
                last[e] = idx
        dmas = [ev for ev in self.dma_sem_last if ev is not None]
        for e in ENGS:
            waits = []
            for f, idx in last.items():
                if f == e:
                    continue
                if self.clock[e].get(f, -1) < idx:
                    waits.append(("c", f, idx))
                    self.ins[f][idx]["marked"] = True
                    self.clock[e][f] = idx
            for ev in dmas:
                if ev[1] not in self.dma_seen[e]:
                    waits.append(ev)
                    self.dma_seen[e].add(ev[1])
            self.ins[e].append({"fn": None, "waits": waits, "marked": False, "dma": None, "clk": dict(self.clock[e])})
        for e in ENGS:
            for f, idx in last.items():
                self.clock[e][f] = max(self.clock[e].get(f, -1), idx)
        self.lastw = {}
        self.readers = {}

    def emit(self):
        nc = self.nc
        waits = []
        for ev in self.final_dma:
            if ev[1] not in self.dma_seen["sp"]:
                waits.append(ev)
        for e in ENGS:
            if e != "sp":
                idx = len(self.ins[e]) - 1
                while idx >= 0 and (self.ins[e][idx]["dma"] is not None or self.ins[e][idx]["fn"] is None):
                    idx -= 1
                if idx >= 0:
                    self.ins[e][idx]["marked"] = True
                    waits.append(("c", e, idx))
        self.ins["sp"].append({"fn": None, "waits": waits, "marked": False, "dma": None, "clk": {}})
        semval = {}
        for e in ENGS:
            c = 0
            for i, rec in enumerate(self.ins[e]):
                if rec["marked"]:
                    c += 1
                    semval[(e, i)] = c
        with contextlib.ExitStack() as st:
            esem = {e: st.enter_context(nc.semaphore("s_" + e)) for e in ENGS}
            dsem = [st.enter_context(nc.semaphore("d_%d" % i)) for i in range(N_DMA_SEMS)]
            block = st.enter_context(nc.Block())
            engobj = {"pe": block.tensor, "act": block.scalar, "dve": block.vector,
                      "pool": block.gpsimd, "sp": block.sync}

            def make(e):
                def body(eng):
                    for i, rec in enumerate(self.ins[e]):
                        for ev in rec["waits"]:
                            if ev[0] == "c":
                                eng.wait_ge(esem[ev[1]], semval[(ev[1], ev[2])])
                            else:
                                eng.wait_ge(dsem[ev[2]], ev[3])
                        if rec["fn"] is None:
                            continue
                        r = rec["fn"](eng)
                        if rec["dma"] is not None:
                            r.then_inc(dsem[rec["dma"][2]], 16)
                        elif rec["marked"]:
                            r.then_inc(esem[e], 1)
                return body
            for e in ENGS:
                engobj[e](make(e))
        return {e: len(self.ins[e]) for e in ENGS}


def _consts():
    ident = np.eye(128, dtype=np.float32)
    maskB = np.zeros((4, 128, 128), np.float32)
    for q in range(4):
        for gl in range(2):
            gq = 2 * q + gl
            maskB[q, gl * 64:(gl + 1) * 64, gq * 16:(gq + 1) * 16] = 1.0
    maskC = np.ascontiguousarray(maskB.transpose(0, 2, 1))
    R = np.zeros((64, 64), np.float32)
    for blk in range(2):
        for d in range(16):
            R[blk * 32 + d, blk * 32 + d + 16] = -1.0
            R[blk * 32 + d + 16, blk * 32 + d] = 1.0
    rotT = np.ascontiguousarray(R.T)
    rows = T_LAT // 64
    row = np.repeat(np.arange(rows), 64).astype(np.float32)
    col = np.tile(np.arange(64), rows).astype(np.float32)
    inv = (10000.0 ** (-np.arange(0, 32, 2, dtype=np.float32) / 32)).astype(np.float32)
    ang_r = row[None, :] * inv[:, None]
    ang_c = col[None, :] * inv[:, None]
    cosF = np.concatenate([np.cos(ang_r), np.cos(ang_r), np.cos(ang_c), np.cos(ang_c)], 0).astype(np.float32)
    sinF = np.concatenate([np.sin(ang_r), np.sin(ang_r), np.sin(ang_c), np.sin(ang_c)], 0).astype(np.float32)
    return {"k_ident": ident, "k_maskB": maskB, "k_maskC": maskC, "k_rotT": rotT,
            "k_cosF": np.ascontiguousarray(cosF), "k_sinF": np.ascontiguousarray(sinF)}


IN_SHAPES = {
    "x": [T_LAT, D], "c": [1, D], "ctx": [T_CTX, D], "c_ctx": [1, D],
    "ada_w": [4, D, 6 * D], "ada_b": [4, 6 * D], "norm_mix_g": [4, D], "norm_ffn_g": [4, D],
    "ssm_a_re": [2, 2, 64, 64], "ssm_a_im": [2, 2, 64, 64], "ssm_log_dt": [2, 2, 64],
    "ssm_b_re": [2, 2, 64, 64, 16], "ssm_b_im": [2, 2, 64, 64, 16],
    "ssm_c_re": [2, 2, 64, 16, 64], "ssm_c_im": [2, 2, 64, 16, 64],
    "ssm_d": [2, D], "ssm_glu_w": [2, D, 2 * D], "ssm_glu_b": [2, 2 * D],
    "attn_w_qkv": [2, D, 1536], "attn_q_g": [2, 64], "attn_k_g": [2, 64], "attn_w_o": [2, D, D],
    "ffn_w_gate_up": [2, D, 2 * DFF], "ffn_w_down": [2, DFF, D],
    "moe_router_w": [2, D, NE], "moe_router_b": [2, NE],
    "moe_w_gate_up": [2, NE, D, 2 * DFF], "moe_w_down": [2, NE, DFF, D],
    "k_ident": [128, 128], "k_maskB": [4, 128, 128], "k_maskC": [4, 128, 128], "k_rotT": [64, 64],
    "k_cosF": [64, T_LAT], "k_sinF": [64, T_LAT],
}


def build(stop_after=None):
    nc = bass.Bass("TRN2", target_bir_lowering=False)
    dr = {k: nc.dram_tensor(k, s, F32, kind="ExternalInput").ap() for k, s in IN_SHAPES.items()}
    out_d = nc.dram_tensor("out", [T_LAT, D], F32, kind="ExternalOutput").ap()
    xT = nc.dram_tensor("xT_scr", [NCH, 128, T], F32, kind="Internal").ap()
    DBG = stop_after is not None
    K = Emitter(nc)
    st = contextlib.ExitStack()

    def sb(name, shape, dt=F32):
        return st.enter_context(nc.sbuf_tensor(name, shape, dt))

    ps = [st.enter_context(nc.psum_tensor("ps%d" % i, [128, 512], F32)) for i in range(8)]
    PS = ["ps%d" % i for i in range(8)]

    hT = sb("hT", [128, NCH, T], BF16)
    ident = sb("ident", [128, 128])
    ones_b = sb("ones_b", [128, 128], BF16)
    ones_f = sb("ones_f", [128, 512])
    iota_f = sb("iota_f", [128, 512])
    iota_i = sb("iota_i", [128, 512], I32)
    modv = sb("modv", [128, 4, 6, NCH, 2])
    Amix = sb("Amix", [128, 4, NCH, 2]); Affn = sb("Affn", [128, 4, NCH, 2])
    gmix = sb("gmix", [128, 4, NCH]); gffn = sb("gffn", [128, 4, NCH])
    adab = sb("adab", [128, 4, 48])
    csil = sb("csil", [128, NCH, 2]); csil_b = sb("csil_b", [128, NCH, 2], BF16)
    epsb = sb("epsb", [128, 1])
    NA = 32256
    ARENA = sb("ARENA", [128, NA])

    class Al:
        def __init__(self):
            self.o = 0

        def f(self, n):
            a = ARENA[:, self.o:self.o + n]
            self.o += n
            assert self.o <= NA, self.o
            return a

        def b(self, n):
            w = (n + 1) // 2
            return self.f(w).bitcast(BF16)[:, 0:n]

        def i(self, n):
            return self.f(n).bitcast(I32)

    def dmaq(i):
        return "sp" if i % 2 == 0 else "act"

    K.dma("sp", ident[:], dr["k_ident"][:, :], writes=["ident"])
    K.op("dve", lambda e: e.memset(ones_b[:], 1.0), writes=["ones_b"])
    K.op("dve", lambda e: e.memset(ones_f[:], 1.0), writes=["ones_f"])
    K.op("dve", lambda e: e.memset(epsb[:], EPS), writes=["epsb"])
    K.op("pool", lambda e: e.iota(iota_i[:], pattern=[[1, 512]], base=1, channel_multiplier=0), writes=["iota_i"])
    K.op("dve", lambda e: e.tensor_copy(out=iota_f[:], in_=iota_i[:]), reads=["iota_i"], writes=["iota_f"])

    al = Al()
    xin = [al.f(1024) for i in range(2)]
    xo = [al.f(1024).rearrange("p (c t) -> p c t", c=NCH) for i in range(2)]
    for blk in range(T // 128):
        b2 = blk % 2
        src = dr["ctx"][blk * 128:(blk + 1) * 128, :] if blk < 2 else dr["x"][(blk - 2) * 128:(blk - 1) * 128, :]
        K.dma(dmaq(blk), xin[b2], src, writes=[("xin", b2)])
        for half in range(2):
            bank = 2 * b2 + half
            for j in range(4):
                c = half * 4 + j
                K.op("pe", lambda e, bank=bank, j=j, c=c, b2=b2: e.transpose(
                    ps[bank][:, j * 128:(j + 1) * 128], xin[b2][:, c * 128:(c + 1) * 128], ident[:]),
                    reads=[("xin", b2), "ident"], writes=[PS[bank]])
            eng = "act" if half == 0 else "dve"
            if eng == "act":
                K.op("act", lambda e, bank=bank, half=half, b2=b2: e.activation(
                    out=xo[b2][:, half * 4:(half + 1) * 4, :], in_=ps[bank][:].rearrange("p (c t) -> p c t", c=4), func=AF.Copy),
                    reads=[PS[bank]], writes=[("xo", b2, half)])
            else:
                K.op("dve", lambda e, bank=bank, half=half, b2=b2: e.tensor_copy(
                    out=xo[b2][:, half * 4:(half + 1) * 4, :], in_=ps[bank][:].rearrange("p (c t) -> p c t", c=4)),
                    reads=[PS[bank]], writes=[("xo", b2, half)])
        K.dma(dmaq(blk + 1), xT[:, :, blk * 128:(blk + 1) * 128].rearrange("c p t -> p c t"), xo[b2],
              reads=[("xo", b2, 0), ("xo", b2, 1)], writes=[("xT", blk // 4 if blk >= 2 else -1)])
    K.fence()

    cs_f = csil
    K.dma("sp", cs_f[:, :, 0], dr["c"][0, :].rearrange("(k p) -> p k", p=128), writes=["csil"], allow_slow_non_contiguous=True)
    K.dma("sp", cs_f[:, :, 1], dr["c_ctx"][0, :].rearrange("(k p) -> p k", p=128), writes=["csil"], allow_slow_non_contiguous=True)
    K.op("act", lambda e: e.activation(out=csil_b[:], in_=csil[:], func=AF.Silu), reads=["csil"], writes=["csil_b"])
    K.dma("sp", adab[:], dr["ada_b"].rearrange("l (j p) -> p l j", p=128), writes=["adab"], allow_slow_non_contiguous=True)
    K.dma("sp", gmix[:], dr["norm_mix_g"].rearrange("l (j p) -> p l j", p=128), writes=["gmix"], allow_slow_non_contiguous=True)
    K.dma("sp", gffn[:], dr["norm_ffn_g"].rearrange("l (j p) -> p l j", p=128), writes=["gffn"], allow_slow_non_contiguous=True)
    al = Al()
    wada = [al.b(8192).rearrange("p (k o) -> p k o", k=NCH) for i in range(2)]
    it = 0
    for l in range(4):
        for m in range(6):
            w2 = it % 2
            K.dma("pool", wada[w2], dr["ada_w"][l, :, m * 1024:(m + 1) * 1024].rearrange("(k p) o -> p k o", p=128),
                  writes=[("wada", w2)])
            bank = it % 2
            for cc in range(NCH):
                for k in range(NCH):
                    K.op("pe", lambda e, bank=bank, cc=cc, k=k, w2=w2: e.matmul(
                        ps[bank][:, cc * 2:cc * 2 + 2], lhsT=wada[w2][:, k, cc * 128:(cc + 1) * 128], rhs=csil_b[:, k, :],
                        start=(k == 0), stop=(k == NCH - 1)), reads=[("wada", w2), "csil_b"], writes=[PS[bank]])
            for col in range(2):
                K.op("dve", lambda e, bank=bank, l=l, m=m, col=col: e.tensor_tensor(
                    out=modv[:, l, m, :, col], in0=ps[bank][:, 0:16].rearrange("p (c t) -> p c t", t=2)[:, :, col],
                    in1=adab[:, l, m * 8:(m + 1) * 8], op=ALU.add), reads=[PS[bank], "adab"], writes=["modv"])
            it += 1
    for l in range(4):
        for col in range(2):
            K.op("dve", lambda e, l=l, col=col: e.scalar_tensor_tensor(
                out=Amix[:, l, :, col], in0=modv[:, l, 1, :, col], scalar=1.0, in1=gmix[:, l, :], op0=ALU.add, op1=ALU.mult),
                reads=["modv", "gmix"], writes=["Amix"])
            K.op("dve", lambda e, l=l, col=col: e.scalar_tensor_tensor(
                out=Affn[:, l, :, col], in0=modv[:, l, 4, :, col], scalar=1.0, in1=gffn[:, l, :], op0=ALU.add, op1=ALU.mult),
                reads=["modv", "gffn"], writes=["Affn"])
    K.fence()

    def norm_phase(l, A, shift_m, router=None):
        al = Al()
        xt = [al.f(4096).rearrange("p (c t) -> p c t", c=NCH) for i in range(2)]
        sq = [al.b(4096).rearrange("p (c t) -> p c t", c=NCH) for i in range(2)]
        ms = al.f(512); rinv = al.f(512); rstd = al.f(512)
        tmp = [al.f(512) for i in range(2)]
        for ti, (t0, n, col) in enumerate(TILES):
            b2 = ti % 2
            K.dma(dmaq(ti), xt[b2][:, :, :n], xT[:, :, t0:t0 + n].rearrange("c p t -> p c t"),
                  reads=[("xT", ti - 1)], writes=[("xt", b2)])
            K.op("act", lambda e, b2=b2, n=n: e.activation(out=sq[b2][:, :, :n], in_=xt[b2][:, :, :n], func=AF.Square),
                 reads=[("xt", b2)], writes=[("sq", b2)])
            for c in range(NCH):
                K.op("pe", lambda e, b2=b2, n=n, c=c: e.matmul(ps[b2][:, :n], lhsT=ones_b[:], rhs=sq[b2][:, c, :n],
                                                             start=(c == 0), stop=(c == NCH - 1)),
                     reads=[("sq", b2), "ones_b"], writes=[PS[b2]])
            K.op("dve", lambda e, b2=b2, n=n: e.tensor_scalar(out=ms[:, :n], in0=ps[b2][:, :n], scalar1=1.0 / D, scalar2=EPS,
                                                              op0=ALU.mult, op1=ALU.add), reads=[PS[b2]], writes=["ms"])
            K.op("dve", lambda e, n=n: e.reciprocal(out=rinv[:, :n], in_=ms[:, :n]), reads=["ms"], writes=["rinv"])
            K.op("act", lambda e, n=n: e.activation(out=rstd[:, :n], in_=rinv[:, :n], func=AF.Sqrt), reads=["rinv"], writes=["rstd"])
            for c in range(NCH):
                c2 = c % 2
                K.op("dve", lambda e, b2=b2, n=n, c=c, c2=c2, col=col: e.scalar_tensor_tensor(
                    out=tmp[c2][:, :n], in0=xt[b2][:, c, :n], scalar=A[:, l, c, col:col + 1], in1=rstd[:, :n],
                    op0=ALU.mult, op1=ALU.mult), reads=[("xt", b2), "rstd"], writes=[("tmp", c2)])
                K.op("act", lambda e, n=n, c=c, c2=c2, col=col, t0=t0: e.activation(
                    out=hT[:, c, t0:t0 + n], in_=tmp[c2][:, :n], func=AF.Identity,
                    bias=modv[:, l, shift_m, c, col:col + 1], scale=1.0), reads=[("tmp", c2)], writes=[("hT", ti)])
        K.fence()

    def resid_update(ti, t0, n, col, c, psb, gate_m, l, xold, xnew, kx):
        K.op("dve", lambda e: e.scalar_tensor_tensor(
            out=xnew[:, c, :n], in0=ps[psb][:, :n], scalar=modv[:, l, gate_m, c, col:col + 1], in1=xold[:, c, :n],
            op0=ALU.mult, op1=ALU.add), reads=[PS[psb], kx], writes=[kx + ("n",)])

    def s5_phase(l):
        j = l // 2
        al = Al()
        fa = al.f
        fb = al.b
        BC = [[fa(128) for _ in range(4)] for _ in range(2)]
        mkB = fa(512); mkC = fa(512)
        PRM0 = al.o
        prm = {nm: fa(64) for nm in ["a_re", "a_im", "ldt", "dt", "mag", "th", "thn", "t0", "t1", "t2", "sn", "cs",
                                      "lre", "lim", "den", "rden", "nr", "f_re", "f_im", "thn2"]}
        prm_i = al.i(64)
        dvec = fa(8)
        yacc = fa(T)
        tabC = [fa(512) for _ in range(4)]; tabS = [fa(512) for _ in range(4)]; tabM = [fa(512) for _ in range(4)]
        tw = [fa(512) for _ in range(10)]
        tw2 = [fa(512) for _ in range(10)]
        tint = al.i(512)
        init = fa(8 * 4).rearrange("p (g k) -> p g k", g=4)
        padt = [fa(128) for _ in range(4)]
        lhs = [[fb(128) for _ in range(5)] for _ in range(4)]
        ub = [[fb(512) for _ in range(4)] for _ in range(2)]

        for gl in range(2):
            sl = slice(gl * 64, (gl + 1) * 64)
            for nm in ("ssm_a_re", "ssm_a_im"):
                K.dma("sp", prm[nm[4:]][sl, :].rearrange("p (r gp) -> p r gp", r=2),
                      dr[nm][j].rearrange("r (gp gl) p -> gl p r gp", gl=2)[gl], writes=[nm[4:]], allow_slow_non_contiguous=True)
            ldt_t = dr["ssm_log_dt"].tensor
            for r in range(2):
                src = bass.AP(ldt_t, j * 128 + r * 64 + gl, [[0, 64], [2, 32]])
                K.dma("sp", prm["ldt"][sl, r * 32:(r + 1) * 32], src, writes=["ldt"], allow_slow_non_contiguous=True)
        K.dma("sp", mkB.rearrange("p (q c) -> p q c", q=4), dr["k_maskB"].rearrange("q p c -> p q c"), writes=["mkB"])
        K.dma("sp", mkC.rearrange("p (q c) -> p q c", q=4), dr["k_maskC"].rearrange("q p c -> p q c"), writes=["mkC"])
        K.dma("sp", dvec, dr["ssm_d"][j].rearrange("(c p) -> p c", p=128), writes=["dvec"], allow_slow_non_contiguous=True)

        P = prm

        def dv(fn, r, w):
            K.op("dve", fn, reads=r, writes=w)

        def frac_sin(src, dst, tag):
            dv(lambda e: e.tensor_copy(out=prm_i, in_=src), [tag], ["prm_i"])
            dv(lambda e: e.tensor_copy(out=P["t0"], in_=prm_i), ["prm_i"], ["t0"])
            dv(lambda e: e.tensor_tensor(out=P["t1"], in0=src, in1=P["t0"], op=ALU.subtract), [tag, "t0"], ["t1"])
            K.op("act", lambda e: e.activation(out=dst, in_=P["t1"], func=AF.Sin, scale=TWO_PI), reads=["t1"], writes=[tag + "_o"])
        K.op("act", lambda e: e.activation(out=P["dt"], in_=P["ldt"], func=AF.Exp), reads=["ldt"], writes=["dt"])
        dv(lambda e: e.tensor_tensor(out=P["t2"], in0=P["a_re"], in1=P["dt"], op=ALU.mult), ["a_re", "dt"], ["t2"])
        K.op("act", lambda e: e.activation(out=P["mag"], in_=P["t2"], func=AF.Exp), reads=["t2"], writes=["mag"])
        dv(lambda e: e.tensor_tensor(out=P["th"], in0=P["a_im"], in1=P["dt"], op=ALU.mult), ["a_im", "dt"], ["th"])
        dv(lambda e: e.tensor_scalar(out=P["thn"], in0=P["th"], scalar1=1.0 / TWO_PI, scalar2=None, op0=ALU.mult), ["th"], ["thn"])
        dv(lambda e: e.tensor_scalar(out=P["thn2"], in0=P["thn"], scalar1=0.25, scalar2=None, op0=ALU.add), ["thn"], ["thn2"])
        frac_sin(P["thn"], P["sn"], "thn")
        frac_sin(P["thn2"], P["cs"], "thn2")
        dv(lambda e: e.tensor_tensor(out=P["lre"], in0=P["mag"], in1=P["cs"], op=ALU.mult), ["mag", "thn2_o"], ["lre"])
        dv(lambda e: e.tensor_tensor(out=P["lim"], in0=P["mag"], in1=P["sn"], op=ALU.mult), ["mag", "thn_o"], ["lim"])
        dv(lambda e: e.tensor_tensor(out=P["t0"], in0=P["a_re"], in1=P["a_re"], op=ALU.mult), ["a_re", "t1"], ["t0"])
        dv(lambda e: e.tensor_tensor(out=P["t1"], in0=P["a_im"], in1=P["a_im"], op=ALU.mult), ["a_im", "t0"], ["t1"])
        dv(lambda e: e.tensor_tensor(out=P["den"], in0=P["t0"], in1=P["t1"], op=ALU.add), ["t0", "t1"], ["den"])
        dv(lambda e: e.reciprocal(out=P["rden"], in_=P["den"]), ["den"], ["rden"])
        dv(lambda e: e.tensor_scalar(out=P["nr"], in0=P["lre"], scalar1=-1.0, scalar2=None, op0=ALU.add), ["lre"], ["nr"])
        dv(lambda e: e.tensor_tensor(out=P["t0"], in0=P["nr"], in1=P["a_re"], op=ALU.mult), ["nr", "a_re", "den"], ["t0"])
        dv(lambda e: e.tensor_tensor(out=P["t1"], in0=P["lim"], in1=P["a_im"], op=ALU.mult), ["lim", "a_im", "den"], ["t1"])
        dv(lambda e: e.tensor_tensor(out=P["t2"], in0=P["t0"], in1=P["t1"], op=ALU.add), ["t0", "t1", "mag"], ["t2"])
        dv(lambda e: e.tensor_tensor(out=P["f_re"], in0=P["t2"], in1=P["rden"], op=ALU.mult), ["t2", "rden"], ["f_re"])
        dv(lambda e: e.tensor_tensor(out=P["t0"], in0=P["lim"], in1=P["a_re"], op=ALU.mult), ["lim", "a_re", "t2"], ["t0"])
        dv(lambda e: e.tensor_tensor(out=P["t1"], in0=P["nr"], in1=P["a_im"], op=ALU.mult), ["nr", "a_im", "t2"], ["t1"])
        dv(lambda e: e.tensor_tensor(out=P["t2"], in0=P["t0"], in1=P["t1"], op=ALU.subtract), ["t0", "t1", "f_re"], ["t2"])
        dv(lambda e: e.tensor_tensor(out=P["f_im"], in0=P["t2"], in1=P["rden"], op=ALU.mult), ["t2", "rden"], ["f_im"])

        (br, bi, q1, q2, q3, q4, rin_re, rin_im, r_re, r_im) = tw

        for cc in range(NCH):
            for r in range(2):
                order = list(range(len(TILES))) if r == 0 else [0] + list(range(len(TILES) - 1, 0, -1))
                bcp = (cc * 2 + r) % 2
                Bre, Bim, Cre, Cim = BC[bcp]
                kbc = ("BC", bcp)
                for gl in range(2):
                    sl = slice(gl * 64, (gl + 1) * 64)
                    for nm, dst in (("ssm_b_re", Bre), ("ssm_b_im", Bim)):
                        K.dma("sp", dst[sl, :].rearrange("p (g i) -> p g i", i=16),
                              dr[nm][j, r, cc * 8:(cc + 1) * 8].rearrange("g p i -> p g i"), writes=[kbc], allow_slow_non_contiguous=True)
                    for nm, dst in (("ssm_c_re", Cre), ("ssm_c_im", Cim)):
                        K.dma("act", dst[:, gl * 64:(gl + 1) * 64],
                              dr[nm][j, r, cc * 8:(cc + 1) * 8].rearrange("g i p -> (g i) p"), writes=[kbc])
                for gpl in range(4):
                    gp = cc * 4 + gpl
                    cb = r * 32 + gp
                    fre = P["f_re"][:, cb:cb + 1]; fim = P["f_im"][:, cb:cb + 1]
                    mB = mkB[:, gpl * 128:(gpl + 1) * 128]; mC = mkC[:, gpl * 128:(gpl + 1) * 128]
                    for which in range(4):
                        pt = padt[which]
                        kp = ("padt", which)
                        if which == 0:
                            dv(lambda e, Bim=Bim, fim=fim, pt=pt: e.tensor_scalar(out=pt, in0=Bim, scalar1=fim, scalar2=None, op0=ALU.mult),
                               [kbc, "f_im"], [kp])
                            dv(lambda e, Bre=Bre, fre=fre, pt=pt: e.scalar_tensor_tensor(out=pt, in0=Bre, scalar=fre, in1=pt, op0=ALU.mult, op1=ALU.subtract),
                               [kbc, "f_re", kp], [kp])
                            dv(lambda e, pt=pt, mB=mB: e.tensor_tensor(out=pt, in0=pt, in1=mB, op=ALU.mult), [kp, "mkB"], [kp])
                        elif which == 1:
                            dv(lambda e, Bre=Bre, fim=fim, pt=pt: e.tensor_scalar(out=pt, in0=Bre, scalar1=fim, scalar2=None, op0=ALU.mult),
                               [kbc, "f_im"], [kp])
                            dv(lambda e, Bim=Bim, fre=fre, pt=pt: e.scalar_tensor_tensor(out=pt, in0=Bim, scalar=fre, in1=pt, op0=ALU.mult, op1=ALU.add),
                               [kbc, "f_re", kp], [kp])
                            dv(lambda e, pt=pt, mB=mB: e.tensor_tensor(out=pt, in0=pt, in1=mB, op=ALU.mult), [kp, "mkB"], [kp])
                        elif which == 2:
                            dv(lambda e, pt=pt, Cre=Cre, mC=mC: e.tensor_tensor(out=pt, in0=Cre, in1=mC, op=ALU.mult), [kbc, "mkC"], [kp])
                        else:
                            dv(lambda e, pt=pt, Cim=Cim, mC=mC: e.tensor_tensor(out=pt, in0=Cim, in1=mC, op=ALU.mult), [kbc, "mkC"], [kp])
                        K.op("pe", lambda e, pt=pt: e.transpose(ps[6][:, 0:128], pt, ident[:]), reads=[kp, "ident"], writes=[PS[6]])
                        K.op("act", lambda e, gpl=gpl, which=which: e.activation(
                            out=lhs[gpl][which], in_=ps[6][:, 0:128], func=AF.Copy, scale=(-1.0 if which == 3 else 1.0)),
                            reads=[PS[6]], writes=[("lhs", gpl, which)])
                        if which == 2:
                            K.op("act", lambda e, gpl=gpl: e.activation(out=lhs[gpl][4], in_=ps[6][:, 0:128], func=AF.Copy, scale=-1.0),
                                 reads=[PS[6]], writes=[("lhs", gpl, 4)])
                    thn = P["thn"][:, cb:cb + 1]
                    T1, T2, T3 = tw[2], tw[3], tw[4]
                    for tab, off, nm in ((tabS[gpl], 0.0, "S"), (tabC[gpl], 0.25, "C")):
                        kt = ("tab", nm, gpl)
                        dv(lambda e: e.tensor_scalar(out=T1, in0=iota_f[:], scalar1=thn, scalar2=off, op0=ALU.mult, op1=ALU.add),
                           ["iota_f", "thn"], [("q1", 0)])
                        dv(lambda e: e.tensor_copy(out=tint, in_=T1), [("q1", 0)], ["tint"])
                        dv(lambda e: e.tensor_copy(out=T2, in_=tint), ["tint"], [("q2", 0)])
                        dv(lambda e: e.tensor_tensor(out=T3, in0=T1, in1=T2, op=ALU.subtract), [("q1", 0), ("q2", 0)], [("q3", 0)])
                        K.op("act", lambda e: e.activation(out=tab, in_=T3, func=AF.Sin, scale=TWO_PI), reads=[("q3", 0)], writes=[kt])
                    K.op("pool", lambda e, gpl=gpl, cb=cb: e.tensor_scalar(out=tabM[gpl], in0=ones_f[:], scalar1=P["mag"][:, cb:cb + 1],
                                                                         scalar2=0.0, op0=ALU.mult, op1=ALU.add),
                         reads=["ones_f", "mag"], writes=[("tab", "M", gpl)])
                for wi, ti in enumerate(order):
                    t0, n, col = TILES[ti]
                    rev = (r == 1)
                    yb = 4 + (wi % 2)
                    for gpl in range(4):
                        b2 = gpl % 2
                        (br, bi, q1, q2, q3, q4, rin_re, rin_im, r_re, r_im) = tw if b2 == 0 else tw2
                        U = ub[b2]

                        def kk(nm):
                            return (nm, b2)
                        Cc = tabC[gpl][:, :n]; Ss = tabS[gpl][:, :n]; Mm = tabM[gpl][:, :n]
                        kC = ("tab", "C", gpl); kS = ("tab", "S", gpl); kM = ("tab", "M", gpl)
                        for half, bank in ((0, 2 * b2), (1, 2 * b2 + 1)):
                            K.op("pe", lambda e: e.matmul(
                                ps[bank][:, :n], lhsT=lhs[gpl][half], rhs=hT[:, cc, t0:t0 + n], start=True, stop=True),
                                reads=[("lhs", gpl, half), ("hT", ti)], writes=[PS[bank]])
                        src_re = ps[2 * b2][:, :n]; src_im = ps[2 * b2 + 1][:, :n]
                        if rev:
                            src_re = src_re[:, ::-1]; src_im = src_im[:, ::-1]
                        K.op("act", lambda e: e.activation(out=br[:, :n], in_=src_re, func=AF.Copy), reads=[PS[2 * b2]], writes=[kk("br")])
                        K.op("act", lambda e: e.activation(out=bi[:, :n], in_=src_im, func=AF.Copy), reads=[PS[2 * b2 + 1]], writes=[kk("bi")])
                        K.op("pool", lambda e: e.tensor_tensor(out=q1[:, :n], in0=br[:, :n], in1=Cc, op=ALU.mult), reads=[kk("br"), kC], writes=[kk("q1")])
                        K.op("pool", lambda e: e.tensor_tensor(out=q2[:, :n], in0=bi[:, :n], in1=Ss, op=ALU.mult), reads=[kk("bi"), kS], writes=[kk("q2")])
                        dv(lambda e: e.tensor_tensor(out=q3[:, :n], in0=bi[:, :n], in1=Cc, op=ALU.mult), [kk("bi"), kC], [kk("q3")])
                        dv(lambda e: e.tensor_tensor(out=q4[:, :n], in0=br[:, :n], in1=Ss, op=ALU.mult), [kk("br"), kS], [kk("q4")])
                        K.op("pool", lambda e: e.tensor_tensor(out=rin_re[:, :n], in0=q1[:, :n], in1=q2[:, :n], op=ALU.add),
                             reads=[kk("q1"), kk("q2")], writes=[kk("rin_re")])
                        dv(lambda e: e.tensor_tensor(out=rin_im[:, :n], in0=q3[:, :n], in1=q4[:, :n], op=ALU.subtract), [kk("q3"), kk("q4")], [kk("rin_im")])
                        ki = ("init", gpl)
                        if wi == 0:
                            dv(lambda e: e.tensor_tensor_scan(out=r_re[:, :n], data0=Mm, data1=rin_re[:, :n], initial=0.0, op0=ALU.mult, op1=ALU.add),
                               [kM, kk("rin_re")], [kk("r_re")])
                            dv(lambda e: e.tensor_tensor_scan(out=r_im[:, :n], data0=Mm, data1=rin_im[:, :n], initial=0.0, op0=ALU.mult, op1=ALU.add),
                               [kM, kk("rin_im")], [kk("r_im")])
                        else:
                            dv(lambda e: e.tensor_tensor_scan(out=r_re[:, :n], data0=Mm, data1=rin_re[:, :n], initial=init[:, gpl, 0:1], op0=ALU.mult, op1=ALU.add),
                               [kM, kk("rin_re"), ki], [kk("r_re")])
                            dv(lambda e: e.tensor_tensor_scan(out=r_im[:, :n], data0=Mm, data1=rin_im[:, :n], initial=init[:, gpl, 1:2], op0=ALU.mult, op1=ALU.add),
                               [kM, kk("rin_im"), ki], [kk("r_im")])
                        if wi < len(order) - 1:
                            e1 = n - 1
                            dv(lambda e: e.tensor_tensor(out=init[:, gpl, 2:3], in0=r_re[:, e1:e1 + 1], in1=tabC[gpl][:, e1:e1 + 1], op=ALU.mult), [kk("r_re"), kC, ki], [ki])
                            dv(lambda e: e.tensor_tensor(out=init[:, gpl, 3:4], in0=r_im[:, e1:e1 + 1], in1=tabS[gpl][:, e1:e1 + 1], op=ALU.mult), [kk("r_im"), kS, ki], [ki])
                            dv(lambda e: e.tensor_tensor(out=init[:, gpl, 4:5], in0=r_re[:, e1:e1 + 1], in1=tabS[gpl][:, e1:e1 + 1], op=ALU.mult), [kk("r_re"), kS, ki], [ki])
                            dv(lambda e: e.tensor_tensor(out=init[:, gpl, 5:6], in0=r_im[:, e1:e1 + 1], in1=tabC[gpl][:, e1:e1 + 1], op=ALU.mult), [kk("r_im"), kC, ki], [ki])
                            dv(lambda e: e.tensor_tensor(out=init[:, gpl, 0:1], in0=init[:, gpl, 2:3], in1=init[:, gpl, 3:4], op=ALU.subtract), [ki], [ki])
                            dv(lambda e: e.tensor_tensor(out=init[:, gpl, 1:2], in0=init[:, gpl, 4:5], in1=init[:, gpl, 5:6], op=ALU.add), [ki], [ki])
                        rr = r_re[:, :n]; ri = r_im[:, :n]; Cx = Cc; Sx = Ss
                        if rev:
                            rr = rr[:, ::-1]; ri = ri[:, ::-1]; Cx = Cx[:, ::-1]; Sx = Sx[:, ::-1]
                        K.op("pool", lambda e: e.tensor_tensor(out=U[0][:, :n], in0=rr, in1=Cx, op=ALU.mult), reads=[kk("r_re"), kC], writes=[("ub", b2, 0)])
                        dv(lambda e: e.tensor_tensor(out=U[1][:, :n], in0=ri, in1=Sx, op=ALU.mult), [kk("r_im"), kS], [("ub", b2, 1)])
                        dv(lambda e: e.tensor_tensor(out=U[2][:, :n], in0=rr, in1=Sx, op=ALU.mult), [kk("r_re"), kS], [("ub", b2, 2)])
                        K.op("pool", lambda e: e.tensor_tensor(out=U[3][:, :n], in0=ri, in1=Cx, op=ALU.mult), reads=[kk("r_im"), kC], writes=[("ub", b2, 3)])
                        for ui, li in ((0, 2), (1, 4), (2, 3), (3, 3)):
                            K.op("pe", lambda e: e.matmul(ps[yb][:, :n], lhsT=lhs[gpl][li], rhs=U[ui][:, :n],
                                                          start=(gpl == 0 and ui == 0), stop=(gpl == 3 and ui == 3)),
                                 reads=[("lhs", gpl, li), ("ub", b2, ui)], writes=[PS[yb]])
                    ky = ("yacc", ti)
                    if r == 0:
                        K.op("act", lambda e, yb=yb, t0=t0, n=n: e.activation(out=yacc[:, t0:t0 + n], in_=ps[yb][:, :n], func=AF.Copy),
                             reads=[PS[yb]], writes=[ky])
                    else:
                        dv(lambda e, yb=yb, t0=t0, n=n: e.tensor_tensor(out=yacc[:, t0:t0 + n], in0=ps[yb][:, :n], in1=yacc[:, t0:t0 + n], op=ALU.add),
                           [PS[yb], ky], [ky])
                        dv(lambda e, t0=t0, n=n: e.scalar_tensor_tensor(out=yacc[:, t0:t0 + n], in0=hT[:, cc, t0:t0 + n], scalar=dvec[:, cc:cc + 1],
                                                                       in1=yacc[:, t0:t0 + n], op0=ALU.mult, op1=ALU.add),
                           [("hT", ti), ky, "dvec"], [ky])
                        K.op("act", lambda e, t0=t0, n=n: e.activation(out=hT[:, cc, t0:t0 + n], in_=yacc[:, t0:t0 + n], func=AF.Gelu),
                             reads=[ky], writes=[("hT", ti)])
        K.fence()
        al = Al()
        xt = [al.f(4096).rearrange("p (c t) -> p c t", c=NCH) for i in range(2)]
        sg = [al.f(512) for i in range(2)]
        oo = [al.f(512) for i in range(2)]
        glub2 = al.f(16)
        wglu = al.b(NCH * 2048).rearrange("p (k o) -> p k o", k=NCH)
        K.dma("sp", glub2, dr["ssm_glu_b"][j].rearrange("(c p) -> p c", p=128), writes=["glub2"], allow_slow_non_contiguous=True)
        K.dma("pool", wglu, dr["ssm_glu_w"][j].rearrange("(k p) o -> p k o", p=128), writes=["wglu"])
        for ti, (t0, n, col) in enumerate(TILES):
            b2 = ti % 2
            kx = ("xt", b2)
            K.dma(dmaq(ti), xt[b2][:, :, :n], xT[:, :, t0:t0 + n].rearrange("c p t -> p c t"), writes=[kx, kx + ("n",)])
            for c in range(NCH):
                c2 = c % 2
                pa, pg = 2 * c2, 2 * c2 + 1
                for k in range(NCH):
                    K.op("pe", lambda e, pa=pa, c=c, k=k, t0=t0, n=n: e.matmul(ps[pa][:, :n], lhsT=wglu[:, k, c * 128:(c + 1) * 128],
                                                                             rhs=hT[:, k, t0:t0 + n], start=(k == 0), stop=(k == NCH - 1)),
                         reads=["wglu", ("hT", ti)], writes=[PS[pa]])
                for k in range(NCH):
                    K.op("pe", lambda e, pg=pg, c=c, k=k, t0=t0, n=n: e.matmul(ps[pg][:, :n], lhsT=wglu[:, k, 1024 + c * 128:1024 + (c + 1) * 128],
                                                                             rhs=hT[:, k, t0:t0 + n], start=(k == 0), stop=(k == NCH - 1)),
                         reads=["wglu", ("hT", ti)], writes=[PS[pg]])
                K.op("act", lambda e, pg=pg, c=c, c2=c2, n=n: e.activation(out=sg[c2][:, :n], in_=ps[pg][:, :n], func=AF.Sigmoid,
                                                                          bias=glub2[:, 8 + c:9 + c], scale=1.0),
                     reads=[PS[pg], "glub2"], writes=[("sg", c2)])
                K.op("dve", lambda e, pa=pa, c=c, c2=c2, n=n: e.scalar_tensor_tensor(out=oo[c2][:, :n], in0=ps[pa][:, :n], scalar=glub2[:, c:c + 1],
                                                                                    in1=sg[c2][:, :n], op0=ALU.add, op1=ALU.mult),
                     reads=[PS[pa], ("sg", c2), "glub2"], writes=[("oo", c2)])
                K.op("dve", lambda e, c=c, c2=c2, n=n, col=col, b2=b2: e.scalar_tensor_tensor(
                    out=xt[b2][:, c, :n], in0=oo[c2][:, :n], scalar=modv[:, l, 2, c, col:col + 1], in1=xt[b2][:, c, :n],
                    op0=ALU.mult, op1=ALU.add), reads=[("oo", c2), kx], writes=[kx + ("n",)])
            K.dma(dmaq(ti + 1), xT[:, :, t0:t0 + n].rearrange("c p t -> p c t"), xt[b2][:, :, :n], reads=[kx, kx + ("n",)], writes=[("xTw", ti)])
        K.fence()

    def attn_phase(l, need_ctx):
        j = l // 2
        for hp in range(2):
            attn_half(l, j, hp, need_ctx)

    def attn_half(l, j, hp, need_ctx):
        al = Al()
        fa, fb = al.f, al.b
        KT = fb(2 * T).rearrange("p (h t) -> p h t", h=2)
        Vs = fb(34 * 2 * 128).rearrange("p (s k d) -> p s k d", s=34, k=2)
        rdb = [fb(512) for _ in range(2)]
        selb = fb(64)
        qh = [fb(512) for _ in range(2)]
        pT = [fb(512) for _ in range(3)]
        sqb = [fb(512) for _ in range(2)]
        qnb = [fb(512) for _ in range(2)]
        Osb = fb(8 * 512).rearrange("p (h t) -> p h t", h=8)
        rotb = fb(64); onesq = fb(64)
        wq = fb(NCH * 768).rearrange("p (k o) -> p k o", k=NCH)
        wo = fb(8 * 1024).rearrange("p (h o) -> p h o", h=8)
        xc = [fa(512) for _ in range(2)]
        cosT = [fa(512) for _ in range(2)]; sinT = [fa(512) for _ in range(2)]
        ms = fa(512); rinv = fa(512); rstd = fa(512); qn = fa(512); t1 = fa(512); t2 = fa(512)
        rden = [fa(512) for _ in range(2)]
        rdh = [fa(512) for _ in range(2)]
        gq = fa(8); rot_f = fa(64)
        K.op("dve", lambda e: e.memset(selb[:, :], 1.0 / 64), writes=["selb"])
        for khl_ in range(2):
            K.op("pool", lambda e: e.memset(Vs[:, :, khl_, 64:128], 1.0), writes=[("Vones", khl_)])

        wsrc = dr["attn_w_qkv"][j]
        K.dma("pool", wq[:, :, 0:512], wsrc[:, hp * 512:(hp + 1) * 512].rearrange("(k p) o -> p k o", p=128), writes=["wq"])
        K.dma("pool", wq[:, :, 512:640], wsrc[:, 1024 + hp * 128:1024 + (hp + 1) * 128].rearrange("(k p) o -> p k o", p=128), writes=["wq"])
        K.dma("pool", wq[:, :, 640:768], wsrc[:, 1280 + hp * 128:1280 + (hp + 1) * 128].rearrange("(k p) o -> p k o", p=128), writes=["wq"])
        K.dma("pool", wo[0:64, :, :], dr["attn_w_o"][j][hp * 512:(hp + 1) * 512, :].rearrange("(h d) o -> d h o", d=64), writes=["wo"])
        K.dma("sp", rot_f[0:64, :], dr["k_rotT"][:, :], writes=["rot_f"])
        K.op("dve", lambda e: e.tensor_copy(out=rotb[0:64, :], in_=rot_f[0:64, :]), reads=["rot_f"], writes=["rotb"])
        K.op("dve", lambda e: e.memset(onesq[0:64, :], 1.0), writes=["onesq"])
        K.dma("sp", gq[0:64, 0:1], dr["attn_q_g"][j].rearrange("(d o) -> d o", o=1), writes=["gq"], allow_slow_non_contiguous=True)
        K.dma("sp", gq[0:64, 1:2], dr["attn_k_g"][j].rearrange("(d o) -> d o", o=1), writes=["gq"], allow_slow_non_contiguous=True)
        K.op("dve", lambda e: e.tensor_scalar(out=gq[0:64, 2:3], in0=gq[0:64, 0:1], scalar1=0.125, scalar2=None, op0=ALU.mult),
             reads=["gq"], writes=["gq2"])

        cnt = [0]

        def proj_stages(wcol, gcol, gkey, ti, t0, n, rope, dst, kdst):
            i = cnt[0] % 2
            cnt[0] += 1
            pq, pn = 2, 3
            tb = ti % 2

            def s0():
                for k in range(NCH):
                    K.op("pe", lambda e: e.matmul(ps[pq][0:64, :n], lhsT=wq[:, k, wcol:wcol + 64], rhs=hT[:, k, t0:t0 + n],
                                                  start=(k == 0), stop=(k == NCH - 1)), reads=["wq", ("hT", ti)], writes=[PS[pq]])
                K.op("act", lambda e: e.activation(out=sqb[i][0:64, :n], in_=ps[pq][0:64, :n], func=AF.Square), reads=[PS[pq]], writes=[("sqb", i)])

            def s1():
                K.op("pe", lambda e: e.matmul(ps[pn][0:64, :n], lhsT=onesq[0:64, :], rhs=sqb[i][0:64, :n], start=True, stop=True),
                     reads=[("sqb", i), "onesq"], writes=[PS[pn]])
                K.op("dve", lambda e: e.tensor_scalar(out=ms[0:64, :n], in0=ps[pn][0:64, :n], scalar1=1.0 / 64, scalar2=EPS, op0=ALU.mult, op1=ALU.add),
                     reads=[PS[pn]], writes=["ms"])
                K.op("dve", lambda e: e.reciprocal(out=rinv[0:64, :n], in_=ms[0:64, :n]), reads=["ms"], writes=["rinv"])
                K.op("act", lambda e: e.activation(out=rstd[0:64, :n], in_=rinv[0:64, :n], func=AF.Sqrt), reads=["rinv"], writes=["rstd"])

            def s2():
                if not rope:
                    K.op("dve", lambda e: e.scalar_tensor_tensor(out=dst, in0=ps[pq][0:64, :n], scalar=gq[0:64, gcol:gcol + 1], in1=rstd[0:64, :n],
                                                                 op0=ALU.mult, op1=ALU.mult), reads=[PS[pq], "rstd", gkey], writes=[kdst])
                    return
                K.op("dve", lambda e: e.scalar_tensor_tensor(out=qn[0:64, :n], in0=ps[pq][0:64, :n], scalar=gq[0:64, gcol:gcol + 1], in1=rstd[0:64, :n],
                                                             op0=ALU.mult, op1=ALU.mult), reads=[PS[pq], "rstd", gkey], writes=["qn"])
                K.op("act", lambda e: e.activation(out=qnb[i][0:64, :n], in_=qn[0:64, :n], func=AF.Copy), reads=["qn"], writes=[("qnb", i)])

            def s3():
                if not rope:
                    return
                K.op("pe", lambda e: e.matmul(ps[pn][0:64, :n], lhsT=rotb[0:64, :], rhs=qnb[i][0:64, :n], start=True, stop=True),
                     reads=[("qnb", i), "rotb"], writes=[PS[pn]])
                K.op("pool", lambda e: e.tensor_tensor(out=t1[0:64, :n], in0=qn[0:64, :n], in1=cosT[tb][0:64, :n], op=ALU.mult),
                     reads=["qn", ("rope", tb)], writes=["t1"])
                K.op("dve", lambda e: e.tensor_tensor(out=t2[0:64, :n], in0=ps[pn][0:64, :n], in1=sinT[tb][0:64, :n], op=ALU.mult),
                     reads=[PS[pn], ("rope", tb)], writes=["t2"])
                K.op("dve", lambda e: e.tensor_tensor(out=dst, in0=t1[0:64, :n], in1=t2[0:64, :n], op=ALU.add), reads=["t1", "t2"], writes=[kdst])
            return [s0, s1, s2, s3]

        def proj_head(*a_):
            for f_ in proj_stages(*a_):
                f_()

        def load_rope(ti, t0, n):
            tb = ti % 2
            p0 = t0 - T_CTX
            K.dma("sp", cosT[tb][0:64, :n], dr["k_cosF"][:, p0:p0 + n], writes=[("rope", tb)])
            K.dma("act", sinT[tb][0:64, :n], dr["k_sinF"][:, p0:p0 + n], writes=[("rope", tb)])

        for ti, (t0, n, col) in enumerate(TILES):
            rope = col == 0
            if rope:
                load_rope(ti, t0, n)
            for khl in range(2):
                proj_head(512 + khl * 64, 1, "gq", ti, t0, n, rope, KT[0:64, khl, t0:t0 + n], ("KT", ti))
            for s_ in range(n // 128):
                sc = t0 // 128 + s_
                pv = 4 + sc % 2
                for k in range(NCH):
                    K.op("pe", lambda e, k=k, sc=sc, pv=pv: e.matmul(ps[pv][:, 0:128], lhsT=hT[:, k, sc * 128:(sc + 1) * 128], rhs=wq[:, k, 640:768],
                                                                   start=(k == 0), stop=(k == NCH - 1)), reads=["wq", ("hT", ti)], writes=[PS[pv]])
                for khl_ in range(2):
                    K.op("act", lambda e: e.activation(out=Vs[:, sc, khl_, 0:64], in_=ps[pv][:, khl_ * 64:(khl_ + 1) * 64], func=AF.Copy),
                         reads=[PS[pv]], writes=[("V", ti)])
        for ti, (t0, n, col) in enumerate(TILES):
            if col == 1 and not need_ctx:
                continue
            rope = col == 0
            if rope:
                load_rope(ti, t0, n)
            s_chunks = list(range(2)) if col == 1 else list(range(34))
            ns = len(s_chunks)
            sched = {4: 0, 10: 1, 16: 2, 22: 3} if ns >= 24 else {}
            nxt = proj_stages(0, 2, "gq2", ti, t0, n, rope, qh[0][0:64, :n], ("qh", 0))
            for f_ in nxt:
                f_()
            for hl in range(8):
                khl = hl // 4
                hb = hl % 2
                po, pd = 4 + hb, 6 + hb
                nxt = None
                if hl + 1 < 8:
                    nxt = proj_stages((hl + 1) * 64, 2, "gq2", ti, t0, n, rope, qh[1 - hb][0:64, :n], ("qh", 1 - hb))

                def qk(si):
                    sc = s_chunks[si]
                    pS = si % 2
                    kt_ti = 0 if sc < 2 else 1 + (sc - 2) // 4
                    K.op("pe", lambda e: e.matmul(ps[pS][:, :n], lhsT=KT[0:64, khl, sc * 128:(sc + 1) * 128], rhs=qh[hb][0:64, :n],
                                                  start=True, stop=True), reads=[("KT", kt_ti), ("qh", hb)], writes=[PS[pS]])
                qk(0)
                if ns > 1:
                    qk(1)
                for si, sc in enumerate(s_chunks):
                    pS = si % 2
                    p3 = si % 3
                    kt_ti = 0 if sc < 2 else 1 + (sc - 2) // 4
                    K.op("act", lambda e: e.activation(out=pT[p3][:, :n], in_=ps[pS][:, :n], func=AF.Exp), reads=[PS[pS]], writes=[("pT", p3)])
                    K.op("pe", lambda e: e.matmul(ps[po][:, :n], lhsT=Vs[:, sc, khl, :], rhs=pT[p3][:, :n],
                                                  start=(si == 0), stop=(si == ns - 1)),
                         reads=[("V", kt_ti), ("Vones", khl), ("pT", p3)], writes=[PS[po]])
                    if si + 2 < ns:
                        qk(si + 2)
                    if nxt is not None and si in sched:
                        nxt[sched[si]]()
                if nxt is not None and not sched:
                    for f_ in nxt:
                        f_()
                K.op("dve", lambda e: e.reciprocal(out=rdh[hb][64:128, :n], in_=ps[po][64:128, :n]), reads=[PS[po]], writes=[("rdh", hb)])
                K.op("act", lambda e: e.activation(out=rdb[hb][64:128, :n], in_=rdh[hb][64:128, :n], func=AF.Copy), reads=[("rdh", hb)], writes=[("rdb", hb)])
                K.op("pe", lambda e: e.matmul(ps[pd][0:64, :n], lhsT=selb[64:128, :], rhs=rdb[hb][64:128, :n], start=True, stop=True),
                     reads=["selb", ("rdb", hb)], writes=[PS[pd]])
                K.op("act", lambda e: e.activation(out=rden[hb][0:64, :n], in_=ps[pd][0:64, :n], func=AF.Copy), reads=[PS[pd]], writes=[("rden", hb)])
                K.op("dve", lambda e: e.tensor_tensor(out=Osb[0:64, hl, :n], in0=ps[po][0:64, :n], in1=rden[hb][0:64, :n], op=ALU.mult),
                     reads=[PS[po], ("rden", hb)], writes=[("Osb", hl)])
            for c in range(NCH):
                pw = 2 + c % 2
                xb2 = c % 2
                kx = ("xc", xb2)
                K.dma(dmaq(c), xc[xb2][:, :n], xT[c, :, t0:t0 + n], reads=[("xTw", ti, c)], writes=[kx])
                for hl in range(8):
                    K.op("pe", lambda e: e.matmul(ps[pw][:, :n], lhsT=wo[0:64, hl, c * 128:(c + 1) * 128], rhs=Osb[0:64, hl, :n],
                                                  start=(hl == 0), stop=(hl == 7)), reads=["wo", ("Osb", hl)], writes=[PS[pw]])
                K.op("dve", lambda e: e.scalar_tensor_tensor(
                    out=xc[xb2][:, :n], in0=ps[pw][:, :n], scalar=modv[:, l, 2, c, col:col + 1], in1=xc[xb2][:, :n],
                    op0=ALU.mult, op1=ALU.add), reads=[PS[pw], kx], writes=[kx])
                K.dma(dmaq(c + 1), xT[c, :, t0:t0 + n], xc[xb2][:, :n], reads=[kx], writes=[("xTw", ti, c)])
        K.fence()

    def ffn_phase(l, moe, need_ctx):
        j = l // 2
        n_exp = NE if moe else 1
        tiles = [(ti,) + TILES[ti] for ti in range(len(TILES)) if need_ctx or TILES[ti][2] == 0]
        passes = [tiles[i:i + 3] for i in range(0, len(tiles), 3)]
        al = Al()
        yacc = al.f(12288).rearrange("p (c t) -> p c t", c=NCH)
        sgt = [al.f(512) for _ in range(2)]
        lg = al.f(8); m8 = al.f(8); ex = al.f(8); mk = al.f(8); gs = al.f(8); nt1 = al.f(8); rgs = al.f(8); gt = al.f(8); rb = al.f(8)
        wblk = [al.b(12288) for _ in range(2)]
        gatesT = al.b(T)
        esel = al.b(NE * 128).rearrange("p (e m) -> p e m", e=NE)
        act_t = [al.b(2048).rearrange("p (f t) -> p f t", f=4) for _ in range(2)]
        act_u = [al.b(512) for _ in range(2)]
        gbc = al.b(1536)
        wr = al.b(NCH * NE).rearrange("p (k e) -> p k e", k=NCH)

        if moe:
            K.dma("pool", wr, dr["moe_router_w"][j].rearrange("(k p) e -> p k e", p=128), writes=["wr"])
            rb_src = bass.AP(dr["moe_router_b"].tensor, j * NE, [[0, 128], [1, NE]])
            K.dma("sp", rb, rb_src, writes=["rb"])
            for ee in range(NE):
                K.op("dve", lambda e: e.tensor_scalar(out=esel[0:8, ee, :], in0=ones_f[0:8, 0:128], scalar1=ident[0:8, ee:ee + 1], scalar2=None,
                                                      op0=ALU.mult), reads=["ident", "ones_f"], writes=["esel"])
            for sc in range(T // 128):
                ti = 0 if sc < 2 else 1 + (sc - 2) // 4
                if TILES[ti][2] == 1 and not need_ctx:
                    continue
                pb = sc % 2
                for k in range(NCH):
                    K.op("pe", lambda e: e.matmul(ps[pb][:, 0:NE], lhsT=hT[:, k, sc * 128:(sc + 1) * 128], rhs=wr[:, k, :],
                                                  start=(k == 0), stop=(k == NCH - 1)), reads=["wr", ("hT", ti)], writes=[PS[pb]])
                K.op("dve", lambda e: e.tensor_tensor(out=lg, in0=ps[pb][:, 0:NE], in1=rb, op=ALU.add), reads=[PS[pb], "rb"], writes=["lg"])
                K.op("dve", lambda e: e.max(out=m8, in_=lg), reads=["lg"], writes=["m8"])
                K.op("dve", lambda e: e.tensor_scalar(out=nt1[:, 0:1], in0=m8[:, 0:1], scalar1=-1.0, scalar2=None, op0=ALU.mult), reads=["m8"], writes=["nt1"])
                K.op("act", lambda e: e.activation(out=ex, in_=lg, func=AF.Exp, bias=nt1[:, 0:1], scale=1.0), reads=["lg", "nt1"], writes=["ex"])
                K.op("dve", lambda e: e.tensor_scalar(out=mk, in0=lg, scalar1=m8[:, 1:2], scalar2=None, op0=ALU.is_ge), reads=["lg", "m8"], writes=["mk"])
                K.op("dve", lambda e: e.tensor_tensor(out=ex, in0=ex, in1=mk, op=ALU.mult), reads=["ex", "mk"], writes=["ex"])
                K.op("dve", lambda e: e.tensor_reduce(out=gs[:, 0:1], in_=ex, axis=mybir.AxisListType.X, op=ALU.add), reads=["ex"], writes=["gs"])
                K.op("dve", lambda e: e.reciprocal(out=rgs[:, 0:1], in_=gs[:, 0:1]), reads=["gs"], writes=["rgs"])
                K.op("dve", lambda e: e.tensor_scalar(out=gt, in0=ex, scalar1=rgs[:, 0:1], scalar2=None, op0=ALU.mult), reads=["ex", "rgs"], writes=["gt"])
                K.op("pe", lambda e: e.transpose(ps[2 + pb][0:8, 0:128], gt, ident[:]), reads=["gt", "ident"], writes=[PS[2 + pb]])
                K.op("act", lambda e: e.activation(out=gatesT[0:8, sc * 128:(sc + 1) * 128], in_=ps[2 + pb][0:8, 0:128], func=AF.Copy),
                     reads=[PS[2 + pb]], writes=[("gatesT", ti)])

        blocks = [(0, 4), (4, 4), (8, 4), (12, 4), (16, 3), (19, 3)]
        wcount = [0]
        for pss in passes:
            offs = []
            a_ = 0
            for t_ in pss:
                offs.append(a_)
                a_ += t_[2]
            first = True
            for ee in range(n_exp):
                if moe:
                    wgu = dr["moe_w_gate_up"][j, ee]; wdn = dr["moe_w_down"][j, ee]
                    for pi, (ti, t0, n, col) in enumerate(pss):
                        K.op("pe", lambda e: e.matmul(ps[6][:, :n], lhsT=esel[0:8, ee, :], rhs=gatesT[0:8, t0:t0 + n], start=True, stop=True),
                             reads=["esel", ("gatesT", ti)], writes=[PS[6]])
                        K.op("act", lambda e: e.activation(out=gbc[:, offs[pi]:offs[pi] + n], in_=ps[6][:, :n], func=AF.Copy),
                             reads=[PS[6]], writes=[("gbc", pi)])
                else:
                    wgu = dr["ffn_w_gate_up"][j]; wdn = dr["ffn_w_down"][j]
                for (f0, nf) in blocks:
                    wb = wcount[0] % 2
                    wcount[0] += 1
                    W = wblk[wb]
                    Wg = W[:, 0:NCH * 512].rearrange("p (k o) -> p k o", k=NCH)
                    Wu = W[:, 4096:4096 + NCH * 512].rearrange("p (k o) -> p k o", k=NCH)
                    Wd = W[:, 8192:12288].rearrange("p (f o) -> p f o", f=4)
                    kw = ("W", wb)
                    K.dma("pool", Wg[:, :, :nf * 128], wgu[:, f0 * 128:(f0 + nf) * 128].rearrange("(k p) o -> p k o", p=128), writes=[kw])
                    K.dma("pool", Wu[:, :, :nf * 128], wgu[:, DFF + f0 * 128:DFF + (f0 + nf) * 128].rearrange("(k p) o -> p k o", p=128), writes=[kw])
                    K.dma("pool", Wd[:, :nf, :], wdn[f0 * 128:(f0 + nf) * 128, :].rearrange("(f p) o -> p f o", p=128), writes=[kw])
                    def GU(pi):
                        ti, t0, n, col = pss[pi]
                        ab = pi % 2
                        o_ = offs[pi]
                        for fi in range(nf):
                            f2 = fi % 2
                            pg, pu = 0 + f2, 2 + f2
                            for k in range(NCH):
                                K.op("pe", lambda e: e.matmul(
                                    ps[pg][:, :n], lhsT=Wg[:, k, fi * 128:(fi + 1) * 128], rhs=hT[:, k, t0:t0 + n], start=(k == 0), stop=(k == NCH - 1)),
                                    reads=[kw, ("hT", ti)], writes=[PS[pg]])
                            for k in range(NCH):
                                K.op("pe", lambda e: e.matmul(
                                    ps[pu][:, :n], lhsT=Wu[:, k, fi * 128:(fi + 1) * 128], rhs=hT[:, k, t0:t0 + n], start=(k == 0), stop=(k == NCH - 1)),
                                    reads=[kw, ("hT", ti)], writes=[PS[pu]])
                            K.op("act", lambda e: e.activation(out=sgt[f2][:, :n], in_=ps[pg][:, :n], func=AF.Silu),
                                 reads=[PS[pg]], writes=[("sgt", f2)])
                            if moe:
                                K.op("dve", lambda e: e.tensor_tensor(out=act_u[f2][:, :n], in0=sgt[f2][:, :n], in1=ps[pu][:, :n], op=ALU.mult),
                                     reads=[("sgt", f2), PS[pu]], writes=[("actu", f2)])
                                K.op("pool", lambda e: e.tensor_tensor(
                                    out=act_t[ab][:, fi, :n], in0=act_u[f2][:, :n], in1=gbc[:, o_:o_ + n], op=ALU.mult),
                                    reads=[("actu", f2), ("gbc", pi)], writes=[("act", ab, fi)])
                            else:
                                K.op("dve", lambda e: e.tensor_tensor(
                                    out=act_t[ab][:, fi, :n], in0=sgt[f2][:, :n], in1=ps[pu][:, :n], op=ALU.mult),
                                    reads=[("sgt", f2), PS[pu]], writes=[("act", ab, fi)])

                    def DOWN(pi, first=first):
                        ti, t0, n, col = pss[pi]
                        ab = pi % 2
                        o_ = offs[pi]
                        for c in range(NCH):
                            py = 4 + c % 2
                            for fi in range(nf):
                                K.op("pe", lambda e: e.matmul(
                                    ps[py][:, :n], lhsT=Wd[:, fi, c * 128:(c + 1) * 128], rhs=act_t[ab][:, fi, :n], start=(fi == 0), stop=(fi == nf - 1)),
                                    reads=[kw, ("act", ab, fi)], writes=[PS[py]])
                            ky = ("yacc", pi, c)
                            if first:
                                K.op("act", lambda e: e.activation(out=yacc[:, c, o_:o_ + n], in_=ps[py][:, :n], func=AF.Copy),
                                     reads=[PS[py]], writes=[ky])
                            else:
                                K.op("dve", lambda e: e.tensor_tensor(out=yacc[:, c, o_:o_ + n], in0=ps[py][:, :n],
                                                                      in1=yacc[:, c, o_:o_ + n], op=ALU.add),
                                     reads=[PS[py], ky], writes=[ky])
                    for pi in range(len(pss) + 1):
                        if pi < len(pss):
                            GU(pi)
                        if pi >= 1:
                            DOWN(pi - 1)
                    first = False
            for pi, (ti, t0, n, col) in enumerate(pss):
                o_ = offs[pi]
                for c in range(NCH):
                    xb2 = c % 2
                    kx = ("xtf", xb2)
                    K.dma(dmaq(c), sgt[xb2][:, :n], xT[c, :, t0:t0 + n], writes=[kx, ("sgt", xb2)])
                    K.op("dve", lambda e: e.scalar_tensor_tensor(
                        out=sgt[xb2][:, :n], in0=yacc[:, c, o_:o_ + n], scalar=modv[:, l, 5, c, col:col + 1], in1=sgt[xb2][:, :n],
                        op0=ALU.mult, op1=ALU.add), reads=[("yacc", pi, c), kx], writes=[kx, ("sgt", xb2)])
                    K.dma(dmaq(c + 1), xT[c, :, t0:t0 + n], sgt[xb2][:, :n], reads=[kx, ("sgt", xb2)], writes=[("xTw", ti, c)])
            K.fence()

    phase = [0]

    def done():
        phase[0] += 1
        return stop_after is not None and phase[0] >= stop_after

    stopped = False
    for l in range(4):
        last = l == 3
        norm_phase(l, Amix, 0)
        if l % 2 == 0:
            s5_phase(l)
        else:
            attn_phase(l, need_ctx=not last)
        if done():
            stopped = True
            break
        norm_phase(l, Affn, 3)
        ffn_phase(l, moe=(l % 2 == 1), need_ctx=not last)
        if done():
            stopped = True
            break

    al = Al()
    xi = [al.f(1024).rearrange("p (c t) -> p c t", c=NCH) for i in range(2)]
    xo2 = [al.f(1024) for i in range(2)]
    for blk in range(T_LAT // 128):
        b2 = blk % 2
        t0 = T_CTX + blk * 128
        K.dma(dmaq(blk), xi[b2], xT[:, :, t0:t0 + 128].rearrange("c p t -> p c t"), writes=[("xi", b2)])
        for half in range(2):
            bank = 2 * b2 + half
            for jj in range(4):
                c = half * 4 + jj
                K.op("pe", lambda e, bank=bank, jj=jj, c=c, b2=b2: e.transpose(ps[bank][:, jj * 128:(jj + 1) * 128], xi[b2][:, c, :], ident[:]),
                     reads=[("xi", b2), "ident"], writes=[PS[bank]])
            if half == 0:
                K.op("act", lambda e, bank=bank, b2=b2: e.activation(out=xo2[b2][:, 0:512], in_=ps[bank][:], func=AF.Copy),
                     reads=[PS[bank]], writes=[("xo2", b2, 0)])
            else:
                K.op("dve", lambda e, bank=bank, b2=b2: e.tensor_copy(out=xo2[b2][:, 512:1024], in_=ps[bank][:]),
                     reads=[PS[bank]], writes=[("xo2", b2, 1)])
        K.dma(dmaq(blk + 1), out_d[blk * 128:(blk + 1) * 128, :], xo2[b2], reads=[("xo2", b2, 0), ("xo2", b2, 1)], final=True)
    counts = K.emit()
    st.close()
    return nc, counts


_CACHE = {}


def kernel(**inputs):
    stop_after = inputs.pop("_stop_after", None)
    n_cores = 4
    key = stop_after
    if key not in _CACHE:
        _CACHE[key] = build(stop_after)
    nc, counts = _CACHE[key]
    consts = _consts()
    in_maps = []
    for b in range(n_cores):
        m = {}
        for k, shp in IN_SHAPES.items():
            if k in consts:
                m[k] = consts[k]
            elif k == "x":
                m[k] = np.ascontiguousarray(inputs["x"][b], dtype=np.float32)
            elif k == "ctx":
                m[k] = np.ascontiguousarray(inputs["ctx"][b], dtype=np.float32)
            elif k == "c":
                m[k] = np.ascontiguousarray(inputs["c"][b:b + 1], dtype=np.float32)
            elif k == "c_ctx":
                m[k] = np.ascontiguousarray(np.asarray(inputs["c_ctx"], dtype=np.float32).reshape(1, D))
            else:
                m[k] = np.ascontiguousarray(inputs[k], dtype=np.float32)
        in_maps.append(m)
    res = run_bass_kernel_spmd(nc, in_maps, core_ids=list(range(n_cores)))
    out = np.stack([np.asarray(res.results[b]["out"], dtype=np.float32) for b in range(n_cores)], axis=0)
    return out
```
